# Optimizing a Trainium2 kernel written in Bass

```python
import jax, jax.numpy as jnp
from jax import lax
import numpy as np

D_MODEL = 1024
BATCH = 16
SEQ = 4096
DEPTH = 4
DEC_BATCH = 32
DEC_SEQ = 2048
PAST_LEN = 128

MIX_W = 1024
GLA_HEADS = 4
GLA_DK = 32
GLA_DV = 64
GLA_W = 256
GLA_GATE_RANK = 16
GLA_GATE_NORMALIZER = 16.0
GLA_CHUNK = 32
FNET_GROUPS = 4
FNET_GROUP_DIM = 64
FNET_W = 256
MLA_HEADS = 8
MLA_Q_LORA = 256
MLA_KV_LORA = 128
MLA_NOPE = 64
MLA_ROPE = 32
MLA_V = 64
MLA_QK = 96
MLA_W = 512
ROPE_THETA = 10000.0
Q_BLOCK = 128
N_GROUPS = 4
EXPERTS_PER_GROUP = 8
N_EXPERTS = 32
TOP_K = 2
D_EXPERT = 512
MOE_BLOCK = 128
EPS = 1e-6
IN_SIZES = (GLA_HEADS * GLA_DK, GLA_HEADS * GLA_DK, GLA_W, GLA_W, GLA_GATE_RANK, GLA_GATE_RANK, FNET_W, MLA_Q_LORA, MLA_KV_LORA, MLA_ROPE)
P_IN = 1472

kernel_name = 'hybrid_bidir_gla_fnet_mla_hmoe'


def rms_norm(x, g):
    xf = x.astype(jnp.float32)
    y = xf * lax.rsqrt(jnp.mean(xf * xf, axis=-1, keepdims=True) + EPS)
    return (y * g.astype(jnp.float32)).astype(x.dtype)


def split_cols(u, sizes):
    idx = np.cumsum(np.array(sizes))[:-1].tolist()
    return jnp.split(u, idx, axis=-1)


def to_heads(t, n_heads):
    B, S, _ = t.shape
    return t.reshape(B, S, n_heads, -1).transpose(0, 2, 1, 3)


def apply_rope(x, pos):
    half = MLA_ROPE // 2
    freqs = ROPE_THETA ** (-jnp.arange(half, dtype=jnp.float32) / half)
    ang = pos.astype(jnp.float32)[:, None] * freqs[None, :]
    cos = jnp.cos(ang).astype(x.dtype)
    sin = jnp.sin(ang).astype(x.dtype)
    x1, x2 = x[..., :half], x[..., half:]
    return jnp.concatenate([x1 * cos - x2 * sin, x1 * sin + x2 * cos], axis=-1)


def gla_chunked(q, k, v, g):
    B, H, S, DK = q.shape
    DV = v.shape[-1]
    C = GLA_CHUNK
    N = S // C
    q = q.reshape(B, H, N, C, DK)
    k = k.reshape(B, H, N, C, DK)
    v = v.reshape(B, H, N, C, DV)
    b = jnp.cumsum(g.reshape(B, H, N, C, DK), axis=3)
    b_last = b[:, :, :, -1:, :]
    q_dec = q * jnp.exp(b)
    k_inv = k * jnp.exp(-b)
    k_end = k * jnp.exp(b_last - b)
    mask = jnp.tril(jnp.ones((C, C), dtype=bool))
    att = jnp.where(mask, jnp.einsum('bhnid,bhnjd->bhnij', q_dec, k_inv), 0.0)
    o_intra = jnp.einsum('bhnij,bhnje->bhnie', att, v)
    d_state = jnp.einsum('bhncd,bhnce->bhnde', k_end, v)
    decay = jnp.exp(b_last[:, :, :, 0, :])

    def step(state, inp):
        dec, ds = inp
        return state * dec[..., None] + ds, state

    init = jnp.zeros((B, H, DK, DV), jnp.float32)
    _, states = lax.scan(step, init, (jnp.moveaxis(decay, 2, 0), jnp.moveaxis(d_state, 2, 0)))
    states = jnp.moveaxis(states, 0, 2)
    o_inter = jnp.einsum('bhncd,bhnde->bhnce', q_dec, states)
    return (o_intra + o_inter).reshape(B, H, S, DV)


def gla_branch(u_q, u_k, u_v, u_og, u_gf, u_gb, up_f, bias_f, up_b, bias_b, out_norm_g):
    B, S, _ = u_q.shape
    f32 = jnp.float32
    q = to_heads(u_q, GLA_HEADS).astype(f32) * (GLA_DK ** -0.5)
    k = to_heads(u_k, GLA_HEADS).astype(f32)
    v = to_heads(u_v, GLA_HEADS).astype(f32)
    g_f = to_heads(jax.nn.log_sigmoid((u_gf @ up_f + bias_f).astype(f32)) / GLA_GATE_NORMALIZER, GLA_HEADS)
    g_b = to_heads(jax.nn.log_sigmoid((u_gb @ up_b + bias_b).astype(f32)) / GLA_GATE_NORMALIZER, GLA_HEADS)
    flip = lambda t: jnp.flip(t, axis=2)
    o_f = gla_chunked(q, k, v, g_f)
    o_b = flip(gla_chunked(flip(q), flip(k), flip(v), flip(g_b)))
    o = o_f + o_b - jnp.sum(q * k, axis=-1, keepdims=True) * v
    o = rms_norm(o, out_norm_g)
    o = o.transpose(0, 2, 1, 3).reshape(B, S, GLA_W).astype(u_og.dtype)
    return o * jax.nn.silu(u_og)


def fourier_branch(u_f):
    B, S, _ = u_f.shape
    xf = u_f.astype(jnp.float32).reshape(B, S, FNET_GROUPS, FNET_GROUP_DIM)
    y = jnp.fft.fft2(xf, axes=(1, 3), norm='ortho').real
    return y.reshape(B, S, FNET_W).astype(u_f.dtype)


def block_attention(q, k, v):
    B, H, S, Dq = q.shape
    Dv = v.shape[-1]
    nb = S // Q_BLOCK
    qb = jnp.moveaxis(q.reshape(B, H, nb, Q_BLOCK, Dq), 2, 0)
    scale = Dq ** -0.5

    def one_block(qi):
        s = jnp.einsum('bhqd,bhkd->bhqk', qi, k).astype(jnp.float32) * scale
        p = jax.nn.softmax(s, axis=-1)
        return jnp.einsum('bhqk,bhkd->bhqd', p.astype(v.dtype), v)

    o = lax.map(one_block, qb)
    return jnp.moveaxis(o, 0, 2).reshape(B, H, S, Dv)


def mla_branch(u_cq, u_ckv, u_kr, pos, q_lora_g, w_uq, kv_lora_g, w_ukv, q_norm_g, k_norm_g):
    B, S, _ = u_cq.shape
    q = to_heads(rms_norm(u_cq, q_lora_g) @ w_uq, MLA_HEADS)
    kv = to_heads(rms_norm(u_ckv, kv_lora_g) @ w_ukv, MLA_HEADS)
    k_nope, v = kv[..., :MLA_NOPE], kv[..., MLA_NOPE:]
    k_rope = jnp.broadcast_to(u_kr[:, None, :, :], (B, MLA_HEADS, S, MLA_ROPE))
    k = jnp.concatenate([k_nope, k_rope], axis=-1)
    q = rms_norm(q, q_norm_g)
    k = rms_norm(k, k_norm_g)
    q = jnp.concatenate([q[..., :MLA_NOPE], apply_rope(q[..., MLA_NOPE:], pos)], axis=-1)
    k = jnp.concatenate([k[..., :MLA_NOPE], apply_rope(k[..., MLA_NOPE:], pos)], axis=-1)
    o = block_attention(q, k, v)
    return o.transpose(0, 2, 1, 3).reshape(B, S, MLA_W)


def hier_moe(h, rg_w, rg_b, re_w, re_b, w1, w3, w2):
    B, S, D = h.shape
    T = B * S
    f32 = jnp.float32
    x = h.reshape(T, D)
    g_prob = jax.nn.softmax((x @ rg_w).astype(f32) + rg_b.astype(f32), axis=-1)
    g_top = jnp.argmax(g_prob, axis=-1)
    g_w = jnp.take_along_axis(g_prob, g_top[:, None], axis=-1)
    e_logits = ((x @ re_w).astype(f32) + re_b.astype(f32)).reshape(T, N_GROUPS, EXPERTS_PER_GROUP)
    e_in = jnp.take_along_axis(e_logits, g_top[:, None, None], axis=1)[:, 0]
    top_l, top_i = lax.top_k(e_in, TOP_K)
    e_w = jax.nn.softmax(top_l, axis=-1) * g_w
    e_id = g_top[:, None].astype(jnp.int32) * EXPERTS_PER_GROUP + top_i.astype(jnp.int32)
    R = T * TOP_K
    flat_e = e_id.reshape(R)
    flat_w = e_w.reshape(R)
    flat_tok = jnp.repeat(jnp.arange(T, dtype=jnp.int32), TOP_K)
    order = jnp.argsort(flat_e)
    se = flat_e[order]
    counts = jnp.bincount(flat_e, length=N_EXPERTS)
    padded = (counts + MOE_BLOCK - 1) // MOE_BLOCK * MOE_BLOCK
    pend = jnp.cumsum(padded)
    pstart = pend - padded
    start = jnp.cumsum(counts) - counts
    dest = pstart[se] + jnp.arange(R, dtype=jnp.int32) - start[se]
    P = (R + MOE_BLOCK - 1) // MOE_BLOCK * MOE_BLOCK + N_EXPERTS * MOE_BLOCK
    n_blk = P // MOE_BLOCK
    row_tok = jnp.full((P,), T, jnp.int32).at[dest].set(flat_tok[order])
    row_w = jnp.zeros((P,), f32).at[dest].set(flat_w[order])
    blk_e = jnp.minimum(jnp.searchsorted(pend, jnp.arange(n_blk, dtype=jnp.int32) * MOE_BLOCK, side='right'), N_EXPERTS - 1)
    x_pad = jnp.concatenate([x, jnp.zeros((1, D), x.dtype)], axis=0)
    xb = x_pad[row_tok].reshape(n_blk, MOE_BLOCK, D)

    def expert_block(args):
        xi, e = args
        hm = jax.nn.silu(xi @ w1[e]) * (xi @ w3[e])
        return hm @ w2[e]

    yb = lax.map(expert_block, (xb, blk_e)).reshape(P, D)
    y = jnp.zeros((T + 1, D), x.dtype).at[row_tok].add(yb * row_w[:, None].astype(x.dtype))[:T]
    return y.reshape(B, S, D)


def trunk(x, c, ada_w, ada_b, norm1_g, norm2_g, w_in, w_out, gla_gate_up_f, gla_gate_bias_f, gla_gate_up_b, gla_gate_bias_b, gla_out_norm_g, mla_q_lora_norm_g, mla_w_uq, mla_kv_lora_norm_g, mla_w_ukv, mla_q_norm_g, mla_k_norm_g, router_group_w, router_group_b, router_expert_w, router_expert_b, expert_w1, expert_w3, expert_w2):
    B, S, D = x.shape
    pos = jnp.arange(S, dtype=jnp.int32)
    c_act = jax.nn.silu(c)
    for l in range(DEPTH):
        mod = (c_act @ ada_w[l] + ada_b[l])[:, None, :]
        sh1, sc1, g1, sh2, sc2, g2 = jnp.split(mod, 6, axis=-1)
        h = rms_norm(x, norm1_g[l]) * (1.0 + sc1) + sh1
        u = h @ w_in[l]
        u_q, u_k, u_v, u_og, u_gf, u_gb, u_f, u_cq, u_ckv, u_kr = split_cols(u, IN_SIZES)
        o_gla = gla_branch(u_q, u_k, u_v, u_og, u_gf, u_gb, gla_gate_up_f[l], gla_gate_bias_f[l], gla_gate_up_b[l], gla_gate_bias_b[l], gla_out_norm_g[l])
        o_fft = fourier_branch(u_f)
        o_mla = mla_branch(u_cq, u_ckv, u_kr, pos, mla_q_lora_norm_g[l], mla_w_uq[l], mla_kv_lora_norm_g[l], mla_w_ukv[l], mla_q_norm_g[l], mla_k_norm_g[l])
        mixed = jnp.concatenate([o_gla, o_fft, o_mla], axis=-1)
        x = x + g1 * (mixed @ w_out[l])
        h = rms_norm(x, norm2_g[l]) * (1.0 + sc2) + sh2
        x = x + g2 * hier_moe(h, router_group_w[l], router_group_b[l], router_expert_w[l], router_expert_b[l], expert_w1[l], expert_w3[l], expert_w2[l])
    return x


def setup_inputs(seed: int = 0) -> dict:
    key = jax.random.key(seed)
    ks = jax.random.split(key, 32)
    f32 = jnp.float32
    D = D_MODEL
    nrm = lambda k, shape, s: jax.random.normal(k, shape, f32) * s
    gain = lambda k, shape: 1.0 + 0.05 * jax.random.normal(k, shape, f32)
    return {
        'x_prompt': nrm(ks[0], (BATCH, SEQ, D), 1.0),
        'x_sample': nrm(ks[1], (DEC_BATCH, DEC_SEQ, D), 1.0),
        'c_prompt': nrm(ks[2], (BATCH, D), 1.0),
        'c_sample': nrm(ks[3], (DEC_BATCH, D), 1.0),
        'ada_w': nrm(ks[4], (DEPTH, D, 6 * D), 0.5 * D ** -0.5),
        'ada_b': nrm(ks[5], (DEPTH, 6 * D), 0.01),
        'norm1_g': gain(ks[6], (DEPTH, D)),
        'norm2_g': gain(ks[7], (DEPTH, D)),
        'w_in': nrm(ks[8], (DEPTH, D, P_IN), D ** -0.5),
        'w_out': nrm(ks[9], (DEPTH, MIX_W, D), MIX_W ** -0.5),
        'gla_gate_up_f': nrm(ks[10], (DEPTH, GLA_GATE_RANK, GLA_HEADS * GLA_DK), GLA_GATE_RANK ** -0.5),
        'gla_gate_bias_f': nrm(ks[11], (DEPTH, GLA_HEADS * GLA_DK), 0.1),
        'gla_gate_up_b': nrm(ks[12], (DEPTH, GLA_GATE_RANK, GLA_HEADS * GLA_DK), GLA_GATE_RANK ** -0.5),
        'gla_gate_bias_b': nrm(ks[13], (DEPTH, GLA_HEADS * GLA_DK), 0.1),
        'gla_out_norm_g': gain(ks[14], (DEPTH, GLA_DV)),
        'mla_q_lora_norm_g': gain(ks[15], (DEPTH, MLA_Q_LORA)),
        'mla_w_uq': nrm(ks[16], (DEPTH, MLA_Q_LORA, MLA_HEADS * MLA_QK), MLA_Q_LORA ** -0.5),
        'mla_kv_lora_norm_g': gain(ks[17], (DEPTH, MLA_KV_LORA)),
        'mla_w_ukv': nrm(ks[18], (DEPTH, MLA_KV_LORA, MLA_HEADS * (MLA_NOPE + MLA_V)), MLA_KV_LORA ** -0.5),
        'mla_q_norm_g': gain(ks[19], (DEPTH, MLA_QK)),
        'mla_k_norm_g': gain(ks[20], (DEPTH, MLA_QK)),
        'router_group_w': nrm(ks[21], (DEPTH, D, N_GROUPS), D ** -0.5),
        'router_group_b': nrm(ks[22], (DEPTH, N_GROUPS), 0.01),
        'router_expert_w': nrm(ks[23], (DEPTH, D, N_EXPERTS), D ** -0.5),
        'router_expert_b': nrm(ks[24], (DEPTH, N_EXPERTS), 0.01),
        'expert_w1': nrm(ks[25], (DEPTH, N_EXPERTS, D, D_EXPERT), D ** -0.5),
        'expert_w3': nrm(ks[26], (DEPTH, N_EXPERTS, D, D_EXPERT), D ** -0.5),
        'expert_w2': nrm(ks[27], (DEPTH, N_EXPERTS, D_EXPERT, D), D_EXPERT ** -0.5),
    }


def reference(x_prompt, x_sample, c_prompt, c_sample, ada_w, ada_b, norm1_g, norm2_g, w_in, w_out, gla_gate_up_f, gla_gate_bias_f, gla_gate_up_b, gla_gate_bias_b, gla_out_norm_g, mla_q_lora_norm_g, mla_w_uq, mla_kv_lora_norm_g, mla_w_ukv, mla_q_norm_g, mla_k_norm_g, router_group_w, router_group_b, router_expert_w, router_expert_b, expert_w1, expert_w3, expert_w2):
    y_prompt = trunk(x_prompt, c_prompt, ada_w, ada_b, norm1_g, norm2_g, w_in, w_out, gla_gate_up_f, gla_gate_bias_f, gla_gate_up_b, gla_gate_bias_b, gla_out_norm_g, mla_q_lora_norm_g, mla_w_uq, mla_kv_lora_norm_g, mla_w_ukv, mla_q_norm_g, mla_k_norm_g, router_group_w, router_group_b, router_expert_w, router_expert_b, expert_w1, expert_w3, expert_w2)
    y_sample = trunk(x_sample, c_sample, ada_w, ada_b, norm1_g, norm2_g, w_in, w_out, gla_gate_up_f, gla_gate_bias_f, gla_gate_up_b, gla_gate_bias_b, gla_out_norm_g, mla_q_lora_norm_g, mla_w_uq, mla_kv_lora_norm_g, mla_w_ukv, mla_q_norm_g, mla_k_norm_g, router_group_w, router_group_b, router_expert_w, router_expert_b, expert_w1, expert_w3, expert_w2)
    return (y_prompt, y_sample)
```

```python
from contextlib import ExitStack
import os
import numpy as np
import ml_dtypes
import concourse.bass as bass
import concourse.mybir as mybir
from concourse.bass_utils import run_bass_kernel_spmd

F32 = mybir.dt.float32
BF16 = mybir.dt.bfloat16
I32 = mybir.dt.int32
ALU = mybir.AluOpType
AF = mybir.ActivationFunctionType
AX = mybir.AxisListType

D = 1024
PU = 1696
EPS = 1e-6
NEXP = 32
BK = 512
BIG = 1.0e4


class Sch:
    def __init__(self, nc, stack):
        self.nc = nc
        self.stack = stack
        self.ops = []
        self.keys = {}
        self.lastop = {}
        self.pending = {e: {} for e in ("pe", "act", "dve", "pool", "sp")}
        self.eng = {"pe": nc.tensor, "act": nc.scalar, "dve": nc.vector, "pool": nc.gpsimd, "sp": nc.sync}

    def op(self, eng, fn, r=(), w=(), chan=None):
        deps = {}

        def add(tok):
            for s_, o_ in tok.items():
                if s_ not in self.eng:
                    o_ = self.lastop[s_]
                if deps.get(s_, -1) < o_:
                    deps[s_] = o_

        for k in r:
            st = self.keys.get(k)
            if st:
                add(st[0])
        for k in w:
            st = self.keys.get(k)
            if st:
                add(st[0])
                add(st[1])
        add(self.pending[eng])
        self.pending[eng] = {}
        src = chan if chan else eng
        oid = len(self.ops)
        self.ops.append((eng, fn, deps, src))
        for k in w:
            self.keys[k] = [{src: oid}, {}]
        for k in r:
            st = self.keys.setdefault(k, [{}, {}])
            st[1][src] = oid
        self.lastop[src] = oid

    def barrier(self):
        for e in self.pending:
            self.pending[e] = dict(self.lastop)
        self.keys.clear()

    def emit(self):
        nc = self.nc
        need = set()
        for (_, _, deps, _) in self.ops:
            need.update(deps.values())
        sems = {}

        def sem(src):
            if src not in sems:
                sems[src] = self.stack.enter_context(nc.semaphore("s_" + str(src)))
            return sems[src]

        semval = {}
        cnt = {}
        known = {e: {} for e in self.eng}
        for oid, (eng, fn, deps, src) in enumerate(self.ops):
            E = self.eng[eng]
            for dsrc, doid in deps.items():
                if dsrc == "pe" and eng == "pe":
                    continue
                v = semval[doid]
                if known[eng].get(dsrc, 0) < v:
                    E.wait_ge(sem(dsrc), v)
                    known[eng][dsrc] = v
            ins = fn(E)
            if src != eng:
                cnt[src] = cnt.get(src, 0) + 16
                ins.then_inc(sem(src), 16)
                semval[oid] = cnt[src]
            elif oid in need:
                cnt[src] = cnt.get(src, 0) + 1
                ins.then_inc(sem(src), 1)
                semval[oid] = cnt[src]
        for src, c in cnt.items():
            nc.sync.wait_ge(sem(src), c)
        return len(self.ops)


class _Stop(Exception):
    pass


def build(seqs, depth, debug=False, upto=99):
    nseq = len(seqs)
    T = sum(seqs)
    NT = T // 128
    NB = (2 * T) // BK + NEXP
    soff = [sum(seqs[:i]) for i in range(nseq)]
    Smax = max(seqs)
    Sset = sorted(set(seqs))

    nc = bass.Bass("TRN2", target_bir_lowering=False)
    stack = ExitStack()
    S = Sch(nc, stack)

    def din(name, shape, dt=F32):
        return nc.dram_tensor(name, list(shape), dt, kind="ExternalInput").ap()

    def dscr(name, shape, dt):
        if debug:
            return nc.dram_tensor(name, list(shape), dt, kind="ExternalOutput").ap()
        return nc.dram_tensor(name, list(shape), dt).ap()

    x_in = din("x_all", [T, D])
    cT_in = din("cT", [D, nseq])
    ada_w = din("ada_w", [depth, D, 6 * D])
    ada_b = din("ada_b", [depth, 6 * D])
    norm1_g = din("norm1_g", [depth, D])
    norm2_g = din("norm2_g", [depth, D])
    w_in = din("w_in", [depth, D, 1472])
    w_out = din("w_out", [depth, D, D])
    up_f = din("gla_gate_up_f", [depth, 16, 128])
    bias_f = din("gla_gate_bias_f", [depth, 128])
    up_b = din("gla_gate_up_b", [depth, 16, 128])
    bias_b = din("gla_gate_bias_b", [depth, 128])
    gla_ng = din("gla_out_norm_g", [depth, 64])
    qlora_g = din("mla_q_lora_norm_g", [depth, 256])
    w_uq = din("mla_w_uq", [depth, 256, 768])
    kvlora_g = din("mla_kv_lora_norm_g", [depth, 128])
    w_ukv = din("mla_w_ukv", [depth, 128, 1024])
    qn_g = din("mla_q_norm_g", [depth, 96])
    kn_g = din("mla_k_norm_g", [depth, 96])
    rg_w = din("router_group_w", [depth, D, 4])
    rg_b = din("router_group_b", [depth, 4])
    re_w = din("router_expert_w", [depth, D, 32])
    re_b = din("router_expert_b", [depth, 32])
    ew1 = din("expert_w1", [depth, 32, D, 512])
    ew3 = din("expert_w3", [depth, 32, D, 512])
    ew2 = din("expert_w2", [depth, 32, 512, D])
    c_f32 = din("c_f32", [128, 1792])
    c_bf = din("c_bf", [128, 1408], BF16)
    c_rope = din("c_rope", [128, Smax // 128, 32])
    c_moe = din("c_moe", [128, 32 + NB + 12 + 12])
    c_chan = din("c_chan", [128, 256], BF16)
    dft = {s_: (din(f"dftc{s_}", [s_, s_], BF16), din(f"dfts{s_}", [s_, s_], BF16)) for s_ in Sset}
    y_out = nc.dram_tensor("y_all", [T, D], F32, kind="ExternalOutput").ap()
    MOD = dscr("MOD", [nseq, 6 * D], F32)
    U = dscr("U", [T, PU], BF16)
    MIXT = dscr("MIXT", [D, T], BF16)
    XM = dscr("XM", [T, D], F32)
    XR = dscr("XR", [T, D], F32)
    H2 = dscr("H2", [T, D], BF16)
    HS = dscr("HS", [NB * BK, D], BF16)
    YB = dscr("YB", [NB * BK, D], BF16)

    uniq = [0]

    def sb(ctx, name, shape, dt):
        uniq[0] += 1
        return ctx.enter_context(nc.sbuf_tensor(f"{name}_{uniq[0]}", list(shape), dt))

    ps = [stack.enter_context(nc.psum_tensor(f"ps{i}", [128, 512], F32)) for i in range(8)]
    PK = [f"ps{i}" for i in range(8)]

    def mm(out, lhsT, rhs, r, w, start=True, stop=True):
        S.op("pe", lambda E: E.matmul(out, lhsT=lhsT, rhs=rhs, start=start, stop=stop), r, w)

    def tr(out, in_, ident, r, w):
        S.op("pe", lambda E: E.transpose(out, in_, ident), r, w)

    def act(out, in_, func, r, w, bias=None, scale=None, accum_out=None):
        kw = {}
        if bias is not None:
            kw["bias"] = bias
        if scale is not None:
            kw["scale"] = scale
        if accum_out is not None:
            kw["accum_out"] = accum_out
        S.op("act", lambda E: E.activation(out, in_, func, **kw), r, w)

    def tt(eng, out, in0, in1, op, r, w):
        S.op(eng, lambda E: E.tensor_tensor(out, in0, in1, op), r, w)

    def ts(eng, out, in0, s1, s2, op0, op1, r, w):
        if op1 is None:
            S.op(eng, lambda E: E.tensor_scalar(out, in0, s1, None, op0), r, w)
        else:
            S.op(eng, lambda E: E.tensor_scalar(out, in0, s1, s2, op0, op1), r, w)

    def stt(out, in0, sc, in1, op0, op1, r, w):
        S.op("dve", lambda E: E.scalar_tensor_tensor(out, in0, sc, in1, op0, op1), r, w)

    def cp(eng, out, in_, r, w):
        if eng == "act":
            S.op("act", lambda E: E.copy(out, in_), r, w)
        else:
            S.op(eng, lambda E: E.tensor_copy(out, in_), r, w)

    def red(out, in_, op, r, w, axis=AX.X):
        S.op("dve", lambda E: E.tensor_reduce(out, in_, axis, op), r, w)

    def recip(out, in_, r, w):
        S.op("dve", lambda E: E.reciprocal(out, in_), r, w)

    def memset(eng, ap, val, w):
        S.op(eng, lambda E: E.memset(ap, val), (), w)

    def dma(q, out, in_, chan, r=(), w=()):
        S.op(q, lambda E: E.dma_start(out=out, in_=in_), r, w, chan=chan)

    def gather(out, in_, idx, chan, r=(), w=()):
        S.op("pool", lambda E: E.indirect_dma_start(out=out, out_offset=None, in_=in_,
                                                     in_offset=bass.IndirectOffsetOnAxis(ap=idx, axis=0)), r, w, chan=chan)

    def scatter(out, in_, idx, chan, r=(), w=()):
        S.op("pool", lambda E: E.indirect_dma_start(out=out, out_offset=bass.IndirectOffsetOnAxis(ap=idx, axis=0),
                                                     in_=in_, in_offset=None), r, w, chan=chan)

    def rstd_of(out, ss, n, r, w):
        act(out, ss, AF.Ln, r, w, scale=1.0 / n, bias=EPS)
        act(out, out, AF.Exp, w, w, scale=-0.5)

    cf = sb(stack, "cf", [128, 1792], F32)
    cb = sb(stack, "cb", [128, 1408], BF16)
    crope = sb(stack, "crope", [128, Smax // 128, 32], F32)
    cmoe = sb(stack, "cmoe", [128, 32 + NB + 24], F32)
    cchan = sb(stack, "cchan", [128, 256], BF16)
    dma("sp", cf[:], c_f32[:, :], "c0", w=["cf"])
    dma("sp", cb[:], c_bf[:, :], "c0", w=["cb"])
    dma("sp", crope[:], c_rope[:, :, :], "c0", w=["crope"])
    dma("sp", cmoe[:], c_moe[:, :], "c0", w=["cmoe"])
    dma("sp", cchan[:], c_chan[:, :], "c0", w=["cchan"])
    identf = cf[:, 0:128]
    Uincl = cf[:, 128:256]
    Urev = cf[:, 256:384]
    onesf = cf[:, 384:512]
    maskf4 = cf[:, 512:1024]
    maskb4 = cf[:, 1024:1536]
    sel65 = cf[0:65, 1536:1600]
    identb = cb[:, 0:128]
    onesb = cb[:, 128:256]
    Lstr = cb[:, 256:384]
    hm4 = cb[:, 384:896]
    hmcol = cb[:, 896:1408]
    CK = ["cf", "cb", "crope", "cmoe", "cchan"]

    cact = sb(stack, "cact", [128, 8, nseq], F32)
    dma("sp", cact[:], cT_in.rearrange("(k p) n -> p k n", p=128), "c0", w=["cact"])
    act(cact[:], cact[:], AF.Silu, ["cact"], ["cact"])
    S.barrier()

    x_src = x_in
    try:
      for l in range(depth):
          x_dst = y_out if l == depth - 1 else XR
          with ExitStack() as cx:
              awb = [sb(cx, f"awb{i}", [128, 8, 512], F32) for i in range(2)]
              modsb = sb(cx, "modsb", [nseq, 6 * D], F32)
              adab = sb(cx, "adab", [nseq, 6 * D], F32)
              g12 = sb(cx, "g12", [nseq, 2 * D], F32)
              dma("sp", adab[:], ada_b[l:l + 1, :].partition_broadcast(nseq), "p0m", w=["adab"])
              dma("sp", g12[:, 0:D], norm1_g[l:l + 1, :].partition_broadcast(nseq), "p0m", w=["g12a"])
              dma("sp", g12[:, D:2 * D], norm2_g[l:l + 1, :].partition_broadcast(nseq), "p0m", w=["g12b"])
              for n in range(12):
                  b_ = n % 2
                  dma("sp", awb[b_][:], ada_w[l].rearrange("(k p) n -> p k n", p=128)[:, :, n * 512:(n + 1) * 512],
                      f"aw{b_}", w=[f"awb{b_}"])
                  pb = PK[n % 2]
                  for k in range(8):
                      mm(ps[n % 2][0:nseq, :], cact[:, k, :], awb[b_][:, k, :], ["cact", f"awb{b_}"], [pb],
                         start=(k == 0), stop=(k == 7))
                  tt("dve", modsb[:, n * 512:(n + 1) * 512], ps[n % 2][0:nseq, :], adab[:, n * 512:(n + 1) * 512],
                     ALU.add, [pb, "adab"], [f"mod{n}"])
              allmod = [f"mod{n}" for n in range(12)]
              stt(modsb[:, D:2 * D], modsb[:, D:2 * D], 1.0, g12[:, 0:D], ALU.add, ALU.mult, allmod + ["g12a"], ["mod2", "mod3"])
              stt(modsb[:, 4 * D:5 * D], modsb[:, 4 * D:5 * D], 1.0, g12[:, D:2 * D], ALU.add, ALU.mult,
                  allmod + ["g12b", "mod2", "mod3"], ["mod8", "mod9"])
              dma("sp", MOD[:, :], modsb[:], "p0s", r=allmod)
          S.barrier()

          if upto <= 0:
              raise _Stop()
          with ExitStack() as cx:
              weff = sb(cx, "weff", [128, 8, PU], BF16)
              with ExitStack() as cx2:
                  wst = sb(cx2, "wst", [128, 4, 1472], F32)
                  gfT = sb(cx2, "gfT", [16, 2, 8, 128], F32)
                  upfb = sb(cx2, "upfb", [16, 2, 128], F32)
                  dma("sp", upfb[:, 0, :], up_f[l], "p1w", w=["upf"])
                  dma("sp", upfb[:, 1, :], up_b[l], "p1w", w=["upb"])
                  for half in range(2):
                      dma("sp", wst[:], w_in[l].rearrange("(k p) n -> p k n", p=128)[:, half * 4:(half + 1) * 4, :],
                          "p1w", w=["wst"])
                      ks = slice(half * 4, half * 4 + 4)
                      cp("dve", weff[:, ks, 0:768], wst[:, :, 0:768], ["wst"], [f"weffA{half}"])
                      cp("pool", weff[:, ks, 1024:PU], wst[:, :, 800:1472], ["wst"], [f"weffB{half}"])
                      for kk in range(4):
                          for d_ in range(2):
                              tr(ps[2 * d_][0:16, kk * 128:(kk + 1) * 128],
                                 wst[:, kk, 768 + 16 * d_:784 + 16 * d_], identf, ["wst", "cf"], [PK[2 * d_]])
                      for d_ in range(2):
                          cp("dve", gfT[:, d_, half * 4:half * 4 + 4, :].rearrange("p k n -> p (k n)"), ps[2 * d_][0:16, :],
                             [PK[2 * d_]], [f"gfT{half}{d_}"])
                      for kk in range(4):
                          k = half * 4 + kk
                          for d_ in range(2):
                              mm(ps[1 + 2 * d_][:, kk * 128:(kk + 1) * 128], gfT[:, d_, k, :], upfb[:, d_, :],
                                 [f"gfT{half}{d_}", "upf", "upb"], [PK[1 + 2 * d_]])
                      for d_ in range(2):
                          cp("dve", weff[:, half * 4:half * 4 + 4, 768 + 128 * d_:896 + 128 * d_],
                             ps[1 + 2 * d_][:].rearrange("p (k n) -> p k n", k=4), [PK[1 + 2 * d_]], [f"weffC{half}{d_}"])
                  S.barrier()
              xt = [sb(cx, f"xt{i}", [128, D], F32) for i in range(2)]
              hb = [sb(cx, f"hb{i}", [128, D], BF16) for i in range(2)]
              hT = [sb(cx, f"hT{i}", [128, 8, 128], BF16) for i in range(2)]
              ub = [sb(cx, f"ub{i}", [128, PU], BF16) for i in range(2)]
              tmpf = sb(cx, "tmpf", [128, D], F32)
              junk = sb(cx, "junk", [128, D], BF16)
              ssq = sb(cx, "ssq", [128, 4], F32)
              modb = [sb(cx, f"modb{i}", [128, 2 * D], F32) for i in range(2)]
              tix = 0
              prev1 = [None]
              for si, Ss in enumerate(seqs):
                  mb = si % 2
                  dma("sp", modb[mb][:], MOD[si:si + 1, 0:2 * D].partition_broadcast(128), f"md{mb}", w=[f"modb{mb}"])
                  def p1gen(t, b_, mb=mb):
                      dma("sp", xt[b_][:], x_src[t * 128:(t + 1) * 128, :], f"xt{b_}", w=[f"xt{b_}"])
                      act(junk[:], xt[b_][:], AF.Square, [f"xt{b_}"], ["junk", f"ssq{b_}"], accum_out=ssq[:, b_:b_ + 1])
                      rstd_of(ssq[:, 2 + b_:3 + b_], ssq[:, b_:b_ + 1], D, [f"ssq{b_}"], [f"rs{b_}"])
                      stt(tmpf[:], xt[b_][:], ssq[:, 2 + b_:3 + b_], modb[mb][:, D:2 * D], ALU.mult, ALU.mult,
                          [f"xt{b_}", f"rs{b_}", f"modb{mb}"], ["tmpf"])
                      tt("pool", hb[b_][:], tmpf[:], modb[mb][:, 0:D], ALU.add, ["tmpf", f"modb{mb}"], [f"hb{b_}"])
                      tpv = ps[4 + b_][:].bitcast(BF16)
                      for k in range(8):
                          tr(tpv[:, k * 128:(k + 1) * 128], hb[b_][:, k * 128:(k + 1) * 128], identb, [f"hb{b_}", "cb"], [PK[4 + b_]])
                      cp("act", hT[b_][:].rearrange("p k n -> p (k n)"), tpv, [PK[4 + b_]], [f"hT{b_}"])
                      yield
                      for n in range(4):
                          c0, c1 = n * 512, min(PU, (n + 1) * 512)
                          for k in range(8):
                              mm(ps[n][:, 0:c1 - c0], hT[b_][:, k, :], weff[:, k, c0:c1], [f"hT{b_}"], [PK[n]],
                                 start=(k == 0), stop=(k == 7))
                          cp("act" if n % 2 == 0 else "dve", ub[b_][:, c0:c1], ps[n][:, 0:c1 - c0], [PK[n]], [f"ub{b_}_{n}"])
                      dma("sp", U[t * 128:(t + 1) * 128, :], ub[b_][:], f"ust{b_}", r=[f"ub{b_}_{n}" for n in range(4)])
                  for tl in range(Ss // 128):
                      t = soff[si] // 128 + tl
                      b_ = tix % 2
                      tix += 1
                      g_ = p1gen(t, b_)
                      next(g_)
                      if prev1[0] is not None:
                          for _ in prev1[0]:
                              pass
                      prev1[0] = g_
              if prev1[0] is not None:
                  for _ in prev1[0]:
                      pass
          S.barrier()

          if upto <= 1:
              raise _Stop()
          with ExitStack() as cx:
              NCm = Smax // 128
              qkT = sb(cx, "qkT", [128, NCm, 512], BF16)
              vall = sb(cx, "vall", [128, NCm, 256], BF16)
              ogall = sb(cx, "ogall", [128, NCm, 256], BF16)
              dsall = sb(cx, "dsall", [128, NCm, 128], F32)
              decall = sb(cx, "decall", [128, NCm, 2], F32)
              stf = sb(cx, "stf", [128, NCm + 1, 128], F32)
              stb16 = sb(cx, "stb16", [128, NCm, 128], BF16)
              ug = [sb(cx, f"ug{i}", [128, 1024], BF16) for i in range(2)]
              gbias = sb(cx, "gbias", [128, 256], F32)
              gng = sb(cx, "gng", [128, 256], F32)
              zt = sb(cx, "zt", [128, 256], F32)
              gt = sb(cx, "gt", [128, 256], F32)
              cs = sb(cx, "cs", [128, 512], F32)
              dd = sb(cx, "dd", [128, 256], F32)
              epos = sb(cx, "epos", [128, 256], F32)
              eneg = sb(cx, "eneg", [128, 256], F32)
              eend = sb(cx, "eend", [128, 256], F32)
              qd = sb(cx, "qd", [128, 4, 128], BF16)
              kef = sb(cx, "kef", [128, 2, 128], F32)
              ke4 = sb(cx, "ke4", [128, 2, 4, 128], BF16)
              m4 = sb(cx, "m4", [128, 4, 4, 128], BF16)
              attm = sb(cx, "attm", [128, 8, 128], BF16)
              osq = sb(cx, "osq", [128, 256], F32)
              o1 = sb(cx, "o1", [128, 256], F32)
              sg = sb(cx, "sg", [128, 256], F32)
              oss = sb(cx, "oss", [128, 8], F32)
              ogl = sb(cx, "ogl", [128, 256], BF16)
              oT = [sb(cx, f"oT{i}", [128, 2, 128], BF16) for i in range(2)]
              dma("sp", gbias[:, 0:128], bias_f[l:l + 1, :].partition_broadcast(128), "p2c", w=["gbias"])
              dma("sp", gbias[:, 128:256], bias_b[l:l + 1, :].partition_broadcast(128), "p2c", w=["gbias"])
              for h in range(4):
                  dma("sp", gng[:, h * 64:(h + 1) * 64], gla_ng[l:l + 1, :].partition_broadcast(128), "p2c", w=["gng"])
              it = 0
              for si, Ss in enumerate(seqs):
                  NCs = Ss // 128
                  t0 = soff[si] // 128
                  for c in range(NCs):
                      t = t0 + c
                      b_ = it % 2
                      it += 1
                      dma("sp", ug[b_][:], U[t * 128:(t + 1) * 128, 0:1024], f"ug{b_}", w=[f"ug{b_}"])
                      cp("pool", vall[:, c, :], ug[b_][:, 256:512], [f"ug{b_}"], [f"v{c}"])
                      cp("pool", ogall[:, c, :], ug[b_][:, 512:768], [f"ug{b_}"], [f"og{c}"])
                      tt("dve", zt[:], ug[b_][:, 768:1024], gbias[:], ALU.add, [f"ug{b_}", "gbias"], ["zt"])
                      act(zt[:], zt[:], AF.Exp, ["zt"], ["zt"], scale=-1.0)
                      act(zt[:], zt[:], AF.Ln, ["zt"], ["zt"], bias=1.0)
                      ts("dve", gt[:], zt[:], -1.0 / 16.0, None, ALU.mult, None, ["zt"], ["gt"])
                      mm(ps[0][:, 0:128], Uincl, gt[:, 0:128], ["gt", "cf"], [PK[0]])
                      mm(ps[0][:, 128:256], Urev, gt[:, 128:256], ["gt", "cf"], [PK[0]])
                      mm(ps[0][:, 256:512], onesf, gt[:], ["gt", "cf"], [PK[0]])
                      mm(ps[1][:, 0:1], gt[:, 0:128], onesf[:, 0:1], ["gt", "cf"], [PK[1]])
                      mm(ps[1][:, 1:2], gt[:, 128:256], onesf[:, 0:1], ["gt", "cf"], [PK[1]])
                      cp("dve", cs[:], ps[0][:], [PK[0]], ["cs"])
                      act(decall[:, c, :], ps[1][:, 0:2], AF.Exp, [PK[1]], [f"dec{c}"])
                      tt("dve", dd[:], cs[:, 256:512], cs[:, 0:256], ALU.subtract, ["cs"], ["dd"])
                      act(epos[:], cs[:, 0:256], AF.Exp, ["cs"], ["epos"])
                      act(eneg[:], cs[:, 0:256], AF.Exp, ["cs"], ["eneg"], scale=-1.0)
                      act(eend[:], dd[:], AF.Exp, ["dd"], ["eend"])
                      qv = ug[b_][:, 0:128]
                      kv_ = ug[b_][:, 128:256]
                      for d_ in range(2):
                          stt(qd[:, 2 * d_, :], qv, 32.0 ** -0.5, epos[:, d_ * 128:(d_ + 1) * 128], ALU.mult, ALU.mult,
                              [f"ug{b_}", "epos"], [f"qd{2 * d_}"])
                          tt("dve", qd[:, 2 * d_ + 1, :], kv_, eneg[:, d_ * 128:(d_ + 1) * 128], ALU.mult,
                             [f"ug{b_}", "eneg"], [f"qd{2 * d_ + 1}"])
                          tt("dve", kef[:, d_, :], kv_, eend[:, d_ * 128:(d_ + 1) * 128], ALU.mult, [f"ug{b_}", "eend"], [f"kef{d_}"])
                          tt("dve", ke4[:, d_, :, :], kef[:, d_:d_ + 1, :].broadcast_to([128, 4, 128]),
                             hmcol.rearrange("p (h n) -> p h n", h=4),
                             ALU.mult, [f"kef{d_}", "cb"], [f"ke4{d_}"])
                      tpv = ps[2][:].bitcast(BF16)
                      for j in range(4):
                          tr(tpv[:, j * 128:(j + 1) * 128], qd[:, j, :], identb, [f"qd{j}", "cb"], [PK[2]])
                      cp("act", qkT[:, c, :], tpv[:, 0:512], [PK[2]], [f"qkT{c}"])
                      for d_ in range(2):
                          for h in range(4):
                              mm(ps[3][:, d_ * 64:(d_ + 1) * 64], ke4[:, d_, h, :], ug[b_][:, 256 + h * 64:256 + (h + 1) * 64],
                                 [f"ke4{d_}", f"ug{b_}"], [PK[3]], start=(h == 0), stop=(h == 3))
                      cp("dve", dsall[:, c, :], ps[3][:, 0:128], [PK[3]], [f"ds{c}"])
                  memset("dve", stf[:, 0, 0:64], 0.0, ["st_f0"])
                  for c in range(NCs):
                      stt(stf[:, c + 1, 0:64], stf[:, c, 0:64], decall[:, c, 0:1], dsall[:, c, 0:64], ALU.mult, ALU.add,
                          [f"st_f{c}", f"dec{c}", f"ds{c}"], [f"st_f{c + 1}"])
                  memset("dve", stf[:, NCs - 1, 64:128], 0.0, [f"st_b{NCs - 1}"])
                  for c in range(NCs - 1, 0, -1):
                      stt(stf[:, c - 1, 64:128], stf[:, c, 64:128], decall[:, c, 1:2], dsall[:, c, 64:128], ALU.mult, ALU.add,
                          [f"st_b{c}", f"dec{c}", f"ds{c}"], [f"st_b{c - 1}"])
                  allst = [f"st_f{c}" for c in range(NCs + 1)] + [f"st_b{c}" for c in range(NCs)]
                  cp("dve", stb16[:, 0:NCs, :], stf[:, 0:NCs, :], allst, ["stb16"])
                  for c in range(NCs):
                      t = t0 + c
                      for j, src in enumerate((1, 3, 0, 2)):
                          tt("dve" if j % 2 == 0 else "pool", m4[:, j, :, :],
                             qkT[:, c, src * 128:(src + 1) * 128].unsqueeze(1).broadcast_to([128, 4, 128]),
                             hm4.rearrange("p (h n) -> p h n", h=4), ALU.mult, [f"qkT{c}", "cb"], [f"m4{j}"])
                      for d_ in range(2):
                          for h in range(4):
                              mm(ps[4 + d_][:, h * 128:(h + 1) * 128], m4[:, d_, h, :], qkT[:, c, (2 * d_) * 128:(2 * d_ + 1) * 128],
                                 [f"m4{d_}", f"qkT{c}"], [PK[4 + d_]])
                          tt("dve", attm[:, d_ * 4:(d_ + 1) * 4, :].rearrange("p h n -> p (h n)"), ps[4 + d_][:],
                             maskf4 if d_ == 0 else maskb4, ALU.mult, [PK[4 + d_], "cf"], [f"attm{d_}"])
                      for h in range(4):
                          o_ = ps[6][:, h * 64:(h + 1) * 64]
                          vh = vall[:, c, h * 64:(h + 1) * 64]
                          mm(o_, attm[:, h, :], vh, ["attm0", f"v{c}"], [PK[6]], start=True, stop=False)
                          mm(o_, attm[:, 4 + h, :], vh, ["attm1", f"v{c}"], [PK[6]], start=False, stop=False)
                          mm(o_, m4[:, 2, h, :], stb16[:, c, 0:64], ["m42", "stb16"], [PK[6]], start=False, stop=False)
                          mm(o_, m4[:, 3, h, :], stb16[:, c, 64:128], ["m43", "stb16"], [PK[6]], start=False, stop=True)
                      act(osq[:], ps[6][:, 0:256], AF.Square, [PK[6]], ["osq"])
                      red(oss[:, 0:4], osq[:].rearrange("p (h n) -> p h n", h=4), ALU.add, ["osq"], ["oss"])
                      rstd_of(oss[:, 4:8], oss[:, 0:4], 64, ["oss"], ["orstd"])
                      tt("dve", o1[:].rearrange("p (h n) -> p h n", h=4), ps[6][:, 0:256].rearrange("p (h n) -> p h n", h=4),
                         oss[:, 4:8].unsqueeze(2).broadcast_to([128, 4, 64]), ALU.mult, [PK[6], "orstd"], ["o1"])
                      act(sg[:], ogall[:, c, :], AF.Silu, [f"og{c}"], ["sg"])
                      tt("pool", o1[:], o1[:], gng[:], ALU.mult, ["o1", "gng"], ["o1"])
                      tt("dve", ogl[:], o1[:], sg[:], ALU.mult, ["o1", "sg"], ["ogl"])
                      tpv = ps[7][:].bitcast(BF16)
                      for j in range(2):
                          tr(tpv[:, j * 128:(j + 1) * 128], ogl[:, j * 128:(j + 1) * 128], identb, ["ogl", "cb"], [PK[7]])
                      ob = c % 2
                      cp("act", oT[ob][:].rearrange("p k n -> p (k n)"), tpv[:, 0:256], [PK[7]], [f"oT{ob}"])
                      dma("sp", MIXT[0:256, t * 128:(t + 1) * 128].rearrange("(k p) n -> p k n", p=128), oT[ob][:], f"oTs{ob}",
                          r=[f"oT{ob}"])
                  S.barrier()
          S.barrier()

          if upto <= 2:
              raise _Stop()
          with ExitStack() as cx:
              NCm = Smax // 128
              xf = sb(cx, "xf", [128, NCm, 256], BF16)
              dbuf = [sb(cx, f"dbuf{i}", [128, NCm, 512], BF16) for i in range(2)]
              ab = sb(cx, "ab", [128, 2, 2, 512], BF16)
              fo = [sb(cx, f"fo{i}", [128, 2, 512], BF16) for i in range(2)]
              it = 0
              for si, Ss in enumerate(seqs):
                  NCs = Ss // 128
                  dma("sp", xf[:, 0:NCs, :], U[soff[si]:soff[si] + Ss, 1024:1280].rearrange("(c p) n -> p c n", p=128), "xf", w=["xf"])
                  for qb in range(Ss // 512):
                      for tg in range(2):
                          dma("sp", dbuf[tg][:, 0:NCs, :],
                              dft[Ss][tg].rearrange("(c p) n -> p c n", p=128)[:, :, qb * 512:(qb + 1) * 512], f"dft{tg}", w=[f"dbuf{tg}"])
                          for mc in range(2):
                              pb = tg * 2 + mc
                              for kc in range(NCs):
                                  mm(ps[pb][:], xf[:, kc, mc * 128:(mc + 1) * 128], dbuf[tg][:, kc, :], ["xf", f"dbuf{tg}"], [PK[pb]],
                                     start=(kc == 0), stop=(kc == NCs - 1))
                              cp("act" if mc == 0 else "dve", ab[:, tg, mc, :], ps[pb][:], [PK[pb]], [f"ab{tg}{mc}"])
                      ob = it % 2
                      it += 1
                      for mc in range(2):
                          mm(ps[4 + mc][:], cchan[:, 0:128], ab[:, 0, mc, :], ["cchan", f"ab0{mc}"], [PK[4 + mc]], start=True, stop=False)
                          mm(ps[4 + mc][:], cchan[:, 128:256], ab[:, 1, mc, :], ["cchan", f"ab1{mc}"], [PK[4 + mc]], start=False, stop=True)
                          cp("act" if mc == 0 else "dve", fo[ob][:, mc, :], ps[4 + mc][:], [PK[4 + mc]], [f"fo{ob}{mc}"])
                      c0 = soff[si] + qb * 512
                      dma("sp", MIXT[256:512, c0:c0 + 512].rearrange("(k p) n -> p k n", p=128), fo[ob][:], f"fos{ob}",
                          r=[f"fo{ob}0", f"fo{ob}1"])
          S.barrier()

          if upto <= 3:
              raise _Stop()
          with ExitStack() as cx:
              NCm = Smax // 128
              kT = sb(cx, "kT", [96, 8, Smax], BF16)
              vext = sb(cx, "vext", [128, NCm, 8, 80], BF16)
              cqT = sb(cx, "cqT", [128, 2, Smax], BF16)
              wuq = sb(cx, "wuq", [128, 2, 768], BF16)
              wukv = sb(cx, "wukv", [128, 1024], BF16)
              wq32 = sb(cx, "wq32", [128, 2, 768], F32)
              gq = sb(cx, "gq", [128, 8, 96], F32)
              gk = sb(cx, "gk", [128, 8, 96], F32)
              gql = sb(cx, "gql", [128, 256], F32)
              gkvl = sb(cx, "gkvl", [128, 128], F32)
              um = [sb(cx, f"um{i}", [128, 416], BF16) for i in range(2)]
              junk = sb(cx, "junk4", [128, 416], F32)
              junkB = sb(cx, "junkB", [128, 768], F32)
              st4 = sb(cx, "st4", [128, 32], F32)
              cqn = sb(cx, "cqn", [128, 384], BF16)
              ckT = sb(cx, "ckT", [128, 128], BF16)
              kfull = sb(cx, "kfull", [128, 8, 96], BF16)
              ktmp = sb(cx, "ktmp", [128, 8, 96], F32)
              rtmp = sb(cx, "rtmp", [128, 4, 8, 16], F32)
              qT = [sb(cx, f"qT{i}", [96, 8, 512], BF16) for i in range(2)]
              pT = [sb(cx, f"pT{i}", [128, 512], BF16) for i in range(4)]
              osb = [sb(cx, f"osb{i}", [65, 512], F32) for i in range(2)]
              mlaT = [sb(cx, f"mlaT{i}", [64, 8, 512], BF16) for i in range(2)]
              dma("sp", wq32[:], w_uq[l].rearrange("(k p) n -> p k n", p=128), "p4c", w=["wq32"])
              cp("dve", wuq[:], wq32[:], ["wq32"], ["wuq"])
              dma("sp", wq32[:, 0, :], w_ukv[l][:, 0:768], "p4c", w=["wq32"], r=["wuq"])
              dma("sp", wq32[:, 1, 0:256], w_ukv[l][:, 768:1024], "p4c", w=["wq32"], r=["wuq"])
              cp("dve", wukv[:, 0:768], wq32[:, 0, :], ["wq32"], ["wukv"])
              cp("dve", wukv[:, 768:1024], wq32[:, 1, 0:256], ["wq32"], ["wukv"])
              for h in range(8):
                  dma("sp", gq[:, h, :], qn_g[l:l + 1, :].partition_broadcast(128), "p4c", w=["gq"])
                  dma("sp", gk[:, h, :], kn_g[l:l + 1, :].partition_broadcast(128), "p4c", w=["gk"])
              dma("sp", gql[:], qlora_g[l:l + 1, :].partition_broadcast(128), "p4c", w=["gql"])
              dma("sp", gkvl[:], kvlora_g[l:l + 1, :].partition_broadcast(128), "p4c", w=["gkvl"])
              memset("dve", vext[:].rearrange("p a b c -> p (a b c)"), 1.0, ["vones"])

              def rope(dst, src, tile_idx, rk, wk):
                  cosb = crope[:, tile_idx, 0:16].unsqueeze(1).broadcast_to([128, 8, 16])
                  sinb = crope[:, tile_idx, 16:32].unsqueeze(1).broadcast_to([128, 8, 16])
                  x1 = src[:, :, 64:80]
                  x2 = src[:, :, 80:96]
                  tt("dve", rtmp[:, 0], x1, cosb, ALU.mult, rk + ["crope"], ["rt0"])
                  tt("pool", rtmp[:, 1], x2, sinb, ALU.mult, rk + ["crope"], ["rt1"])
                  tt("dve", rtmp[:, 2], x1, sinb, ALU.mult, rk + ["crope"], ["rt2"])
                  tt("pool", rtmp[:, 3], x2, cosb, ALU.mult, rk + ["crope"], ["rt3"])
                  tt("dve", dst[:, :, 64:80], rtmp[:, 0], rtmp[:, 1], ALU.subtract, ["rt0", "rt1"], wk)
                  tt("dve", dst[:, :, 80:96], rtmp[:, 2], rtmp[:, 3], ALU.add, ["rt2", "rt3"], wk)

              it = 0
              sbank = 0
              for si, Ss in enumerate(seqs):
                  NCs = Ss // 128
                  t0 = soff[si] // 128
                  for c in range(NCs):
                      t = t0 + c
                      b_ = it % 2
                      it += 1
                      dma("sp", um[b_][:], U[t * 128:(t + 1) * 128, 1280:PU], f"um{b_}", w=[f"um{b_}"])
                      uk = f"um{b_}"
                      act(junk[:, 0:256], um[b_][:, 0:256], AF.Square, [uk], ["junk", "ssa"], accum_out=st4[:, 0:1])
                      act(junk[:, 256:384], um[b_][:, 256:384], AF.Square, [uk], ["junk", "ssb"], accum_out=st4[:, 1:2])
                      act(junk[:, 384:416], um[b_][:, 384:416], AF.Square, [uk], ["junk", "ssc"], accum_out=st4[:, 2:3])
                      rstd_of(st4[:, 4:5], st4[:, 0:1], 256, ["ssa"], ["rsa"])
                      rstd_of(st4[:, 5:6], st4[:, 1:2], 128, ["ssb"], ["rsb"])
                      stt(cqn[:, 0:256], um[b_][:, 0:256], st4[:, 4:5], gql[:], ALU.mult, ALU.mult, [uk, "rsa", "gql"], ["cqn_a"])
                      stt(cqn[:, 256:384], um[b_][:, 256:384], st4[:, 5:6], gkvl[:], ALU.mult, ALU.mult, [uk, "rsb", "gkvl"], ["cqn_b"])
                      if float(os.environ.get('P4CUT', '9')) < 1:
                          continue
                      tpv = ps[7][:].bitcast(BF16)
                      for j in range(3):
                          tr(tpv[:, j * 128:(j + 1) * 128], cqn[:, j * 128:(j + 1) * 128], identb, ["cqn_a", "cqn_b", "cb"], [PK[7]])
                      cp("act", cqT[:, :, c * 128:(c + 1) * 128], tpv[:, 0:256].rearrange("p (k n) -> p k n", k=2), [PK[7]], [f"cqT{c}"])
                      cp("act", ckT[:], tpv[:, 256:384], [PK[7]], ["ckT"])
                      if float(os.environ.get('P4CUT', '9')) < 1.3:
                          continue
                      mm(ps[5][:], ckT[:], wukv[:, 0:512], ["ckT", "wukv"], [PK[5]])
                      mm(ps[6][:], ckT[:], wukv[:, 512:1024], ["ckT", "wukv"], [PK[6]])
                      if float(os.environ.get('P4CUT', '9')) < 1.6:
                          continue
                      for hf in range(2):
                          cp("act", vext[:, c, hf * 4:(hf + 1) * 4, 0:64],
                             ps[5 + hf][:].rearrange("p (h n) -> p h n", h=4)[:, :, 64:128], [PK[5 + hf], "vones"], [f"vx{c}_{hf}"])
                      if float(os.environ.get('P4CUT', '9')) < 2:
                          continue
                      for hf in range(2):
                          act(junkB[:, hf * 256:(hf + 1) * 256].rearrange("p (h n) -> p h n", h=4),
                              ps[5 + hf][:].rearrange("p (h n) -> p h n", h=4)[:, :, 0:64], AF.Square, [PK[5 + hf]], [f"junkk{hf}"])
                      red(st4[:, 8:16], junkB[:, 0:512].rearrange("p (h n) -> p h n", h=8), ALU.add, ["junkk0", "junkk1"], ["ssk"])
                      ts("dve", st4[:, 8:16], st4[:, 8:16], st4[:, 2:3], None, ALU.add, None, ["ssk", "ssc"], ["ssk"])
                      rstd_of(st4[:, 16:24], st4[:, 8:16], 96, ["ssk"], ["rsk"])
                      for hf in range(2):
                          tt("dve", ktmp[:, hf * 4:(hf + 1) * 4, 0:64], ps[5 + hf][:].rearrange("p (h n) -> p h n", h=4)[:, :, 0:64],
                             st4[:, 16 + hf * 4:20 + hf * 4].unsqueeze(2).broadcast_to([128, 4, 64]), ALU.mult, [PK[5 + hf], "rsk"], [f"ktn{hf}"])
                      tt("dve", ktmp[:, :, 64:96], um[b_][:, 384:416].unsqueeze(1).broadcast_to([128, 8, 32]),
                         st4[:, 16:24].unsqueeze(2).broadcast_to([128, 8, 32]), ALU.mult, [uk, "rsk"], ["ktr"])
                      tt("pool", ktmp[:], ktmp[:], gk[:], ALU.mult, ["ktn0", "ktn1", "ktr", "gk"], ["ktg"])
                      if float(os.environ.get('P4CUT', '9')) < 3:
                          continue
                      cp("pool", kfull[:, :, 0:64], ktmp[:, :, 0:64], ["ktg"], ["kfa"])
                      rope(kfull, ktmp, c, ["ktg"], ["kfb"])
                      if float(os.environ.get('P4CUT', '9')) < 4:
                          continue
                      tpv = ps[4][:].bitcast(BF16)
                      for h in range(8):
                          tr(tpv[0:96, h * 128:(h + 1) * 128], kfull[:, h, :], identb, ["kfa", "kfb", "cb"], [PK[4]])
                      cp("act", kT[:, :, c * 128:(c + 1) * 128], tpv[0:96, :].rearrange("p (h n) -> p h n", h=8), [PK[4]], [f"kT{c}"])
                  S.barrier()
                  allk = []
                  allv = []
                  kflat = ktmp[:].rearrange("p h n -> p (h n)")

                  def qprep_gen(qb):
                      qbuf = qb % 2
                      for j in range(4):
                          c = qb * 4 + j
                          for kc in range(2):
                              mm(ps[5][:], cqT[:, kc, c * 128:(c + 1) * 128], wuq[:, kc, 0:512], ["wuq"], [PK[5]],
                                 start=(kc == 0), stop=(kc == 1))
                          for kc in range(2):
                              mm(ps[6][:, 0:256], cqT[:, kc, c * 128:(c + 1) * 128], wuq[:, kc, 512:768], ["wuq"], [PK[6]],
                                 start=(kc == 0), stop=(kc == 1))
                          yield
                          yield
                          cp("dve", kflat[:, 0:512], ps[5][:], [PK[5]], ["qa"])
                          cp("dve", kflat[:, 512:768], ps[6][:, 0:256], [PK[6]], ["qb"])
                          yield
                          yield
                          tt("dve", junkB[:, 0:768], kflat, kflat, ALU.mult, ["qa", "qb"], ["junkq"])
                          yield
                          yield
                          red(st4[:, 8:16], junkB[:, 0:768].rearrange("p (h n) -> p h n", h=8), ALU.add, ["junkq"], ["ssq"])
                          yield
                          yield
                          act(st4[:, 16:24], st4[:, 8:16], AF.Ln, ["ssq"], ["rsq"], scale=1.0 / 96, bias=EPS)
                          yield
                          act(st4[:, 16:24], st4[:, 16:24], AF.Exp, ["rsq"], ["rsq"], scale=-0.5)
                          yield
                          yield
                          tt("dve", ktmp[:], ktmp[:], st4[:, 16:24].unsqueeze(2).broadcast_to([128, 8, 96]), ALU.mult,
                             ["qa", "qb", "rsq"], ["qn"])
                          yield
                          yield
                          tt("pool", ktmp[:], ktmp[:], gq[:], ALU.mult, ["qn", "gq"], ["qg"])
                          yield
                          yield
                          yield
                          cp("pool", kfull[:, :, 0:64], ktmp[:, :, 0:64], ["qg"], ["kfa"])
                          rope(kfull, ktmp, c, ["qg"], ["kfb"])
                          yield
                          yield
                          yield
                          yield
                          tpv_ = ps[4][:].bitcast(BF16)
                          for h in range(8):
                              tr(tpv_[0:96, h * 128:(h + 1) * 128], kfull[:, h, :], identb, ["kfa", "kfb", "cb"], [PK[4]])
                          yield
                          yield
                          yield
                          cp("act", qT[qbuf][:, :, j * 128:(j + 1) * 128], tpv_[0:96, :].rearrange("p (h n) -> p h n", h=8),
                             [PK[4]], [f"qT{qbuf}_{j}"])
                          yield

                  def attn_gen(qb):
                      qbuf = qb % 2
                      qk = [f"qT{qbuf}_{j}" for j in range(4)]
                      steps = [(h, kc) for h in range(8) for kc in range(NCs)]
                      SBK = [0, 1]
                      Dp = 1

                      def qk_mm(i):
                          h_, kc_ = steps[i]
                          bk = SBK[i % len(SBK)]
                          mm(ps[bk][:], kT[:, h_, kc_ * 128:(kc_ + 1) * 128], qT[qbuf][:, h_, :], qk, [PK[bk]])

                      def ep2(h_):
                          a_ = h_ % 2
                          mm(ps[7][0:64, :], sel65, osb[a_][:], ["cf", f"osb{a_}"], [PK[7]])
                          tt("dve", mlaT[qbuf][:, h_, :], osb[a_][0:64, :], ps[7][0:64, :], ALU.mult, [f"osb{a_}", PK[7]],
                             [f"mlaT{qbuf}_{h_}"])

                      for i in range(min(Dp, len(steps))):
                          qk_mm(i)
                      due = {}
                      for i, (h, kc) in enumerate(steps):
                          ab_ = h % 2
                          acc = ps[2 + ab_]
                          bk = SBK[i % len(SBK)]
                          pb_ = i % 4
                          act(pT[pb_][:], ps[bk][:], AF.Exp, [PK[bk]], [f"pT{pb_}"], scale=96.0 ** -0.5)
                          if i + Dp < len(steps):
                              qk_mm(i + Dp)
                          mm(acc[0:65, :], vext[:, kc, h, 0:65], pT[pb_][:], [f"pT{pb_}"], [PK[2 + ab_]],
                             start=(kc == 0), stop=(kc == NCs - 1))
                          if kc == NCs - 1:
                              cp("dve", osb[ab_][:], acc[0:65, :], [PK[2 + ab_]], [f"osb{ab_}"])
                              recip(osb[ab_][64:65, :], osb[ab_][64:65, :], [f"osb{ab_}"], [f"osb{ab_}"])
                              due[i + 4] = h
                          if i in due:
                              ep2(due.pop(i))
                          yield
                      for i_ in sorted(due):
                          ep2(due[i_])
                      c0 = soff[si] + qb * 512
                      dma("sp", MIXT[512:1024, c0:c0 + 512].rearrange("(h p) n -> p h n", p=64), mlaT[qbuf][:], f"mls{qbuf}",
                          r=[f"mlaT{qbuf}_{h}" for h in range(8)])

                  nqb = Ss // 512 if os.environ.get('P4SKIP') != '1' else 0
                  if nqb:
                      for _ in qprep_gen(0):
                          pass
                  for qb in range(nqb):
                      pg = qprep_gen(qb + 1) if qb + 1 < nqb else None
                      for _ in attn_gen(qb):
                          if pg is not None:
                              try:
                                  next(pg)
                              except StopIteration:
                                  pg = None
                      if pg is not None:
                          for _ in pg:
                              pass
                  S.barrier()
          S.barrier()

          if upto <= 4:
              raise _Stop()
          with ExitStack() as cxm:
              m1all = sb(cxm, "m1all", [128, NT, 32], F32)
              m2all = sb(cxm, "m2all", [128, NT, 32], F32)
              rkall = sb(cxm, "rkall", [128, NT, 32], F32)
              w12 = sb(cxm, "w12", [128, NT, 2], F32)
              destf = sb(cxm, "destf", [128, NT, 2], F32)
              desti = sb(cxm, "desti", [128, NT, 2], I32)
              widx = sb(cxm, "widx", [128, NB, 12], I32)
              base = sb(cxm, "base", [128, 32], F32)
              eidx = cmoe[:, 0:32]
              thr = cmoe[:, 32:32 + NB]
              wstride = cmoe[:, 32 + NB:32 + NB + 12]
              wcst = cmoe[:, 32 + NB + 12:32 + NB + 24]
              with ExitStack() as cx:
                  wo32 = sb(cx, "wo32", [128, 4, D], F32)
                  wout = sb(cx, "wout", [128, 8, D], BF16)
                  wr = sb(cx, "wr", [128, 8, 36], F32)
                  rb = sb(cx, "rb", [128, 36], F32)
                  mixb = [sb(cx, f"mixb{i}", [128, 8, 512], BF16) for i in range(2)]
                  xt = [sb(cx, f"xt5{i}", [128, D], F32) for i in range(2)]
                  xn = [sb(cx, f"xn{i}", [128, D], F32) for i in range(2)]
                  h2f = [sb(cx, f"h2f{i}", [128, D], F32) for i in range(2)]
                  h2b = [sb(cx, f"h2b{i}", [128, D], BF16) for i in range(2)]
                  h2T = sb(cx, "h2T", [128, 8, 128], F32)
                  tmp5 = sb(cx, "tmp5", [128, D], F32)
                  junk5 = sb(cx, "junk5", [128, D], BF16)
                  modb = [sb(cx, f"modc{i}", [128, 3 * D], F32) for i in range(2)]
                  s5 = sb(cx, "s5", [128, 16], F32)
                  lg = sb(cx, "lg", [128, 36], F32)
                  lm = sb(cx, "lm", [128, 32], F32)
                  lm2 = sb(cx, "lm2", [128, 32], F32)
                  pen = sb(cx, "pen", [128, 4], F32)
                  gm = sb(cx, "gm", [128, 4], F32)
                  Mb = sb(cx, "Mb", [128, 32], BF16)
                  for half in range(2):
                      dma("sp", wo32[:], w_out[l].rearrange("(k p) n -> p k n", p=128)[:, half * 4:(half + 1) * 4, :], "p5w", w=["wo32"])
                      cp("dve", wout[:, half * 4:(half + 1) * 4, :], wo32[:], ["wo32"], [f"wout{half}"])
                  dma("sp", wr[:, :, 0:4], rg_w[l].rearrange("(k p) n -> p k n", p=128), "p5w", w=["wr"])
                  dma("sp", wr[:, :, 4:36], re_w[l].rearrange("(k p) n -> p k n", p=128), "p5w", w=["wr"])
                  dma("sp", rb[:, 0:4], rg_b[l:l + 1, :].partition_broadcast(128), "p5w", w=["rb"])
                  dma("sp", rb[:, 4:36], re_b[l:l + 1, :].partition_broadcast(128), "p5w", w=["rb"])
                  memset("dve", base[:], 0.0, ["base"])
                  tix = 0
                  prev5 = [None]
                  for si, Ss in enumerate(seqs):
                      mb = si % 2
                      dma("sp", modb[mb][:, 0:D], MOD[si:si + 1, 2 * D:3 * D].partition_broadcast(128), f"mc{mb}", w=[f"modc{mb}"])
                      dma("sp", modb[mb][:, D:3 * D], MOD[si:si + 1, 3 * D:5 * D].partition_broadcast(128), f"mc{mb}", w=[f"modc{mb}"])
                      for qb in range(Ss // 512):
                          xb = (soff[si] // 512 + qb) % 2
                          c0 = soff[si] + qb * 512
                          dma("sp", mixb[xb][:], MIXT[:, c0:c0 + 512].rearrange("(k p) n -> p k n", p=128), f"mixb{xb}", w=[f"mixb{xb}"])
                          def p5gen(t, b_, j, xb=xb, mb=mb):
                              dma("sp", xt[b_][:], x_src[t * 128:(t + 1) * 128, :], f"xt5{b_}", w=[f"xt5{b_}"])
                              for n in range(2):
                                  for k in range(8):
                                      mm(ps[n][:], mixb[xb][:, k, j * 128:(j + 1) * 128], wout[:, k, n * 512:(n + 1) * 512],
                                         [f"mixb{xb}", "wout0", "wout1"], [PK[n]], start=(k == 0), stop=(k == 7))
                              for n in range(2):
                                  tt("dve", tmp5[:, n * 512:(n + 1) * 512], ps[n][:], modb[mb][:, n * 512:(n + 1) * 512], ALU.mult,
                                     [PK[n], f"modc{mb}"], [f"tmp5{n}"])
                              tt("pool", xn[b_][:], tmp5[:], xt[b_][:], ALU.add, ["tmp50", "tmp51", f"xt5{b_}"], [f"xn{b_}"])
                              dma("sp", XM[t * 128:(t + 1) * 128, :], xn[b_][:], f"xns{b_}", r=[f"xn{b_}"])
                              act(junk5[:], xn[b_][:], AF.Square, [f"xn{b_}"], ["junk5", "ss5"], accum_out=s5[:, 0:1])
                              rstd_of(s5[:, 1:2], s5[:, 0:1], D, ["ss5"], ["rs5"])
                              stt(tmp5[:], xn[b_][:], s5[:, 1:2], modb[mb][:, 2 * D:3 * D], ALU.mult, ALU.mult,
                                  [f"xn{b_}", "rs5", f"modc{mb}"], ["tmp50", "tmp51"])
                              tt("dve", h2f[b_][:], tmp5[:], modb[mb][:, D:2 * D], ALU.add, ["tmp50", "tmp51", f"modc{mb}"], [f"h2f{b_}"])
                              cp("pool", h2b[b_][:], h2f[b_][:], [f"h2f{b_}"], [f"h2b{b_}"])
                              dma("sp", H2[t * 128:(t + 1) * 128, :], h2b[b_][:], f"h2s{b_}", r=[f"h2b{b_}"])
                              yield
                              for k in range(8):
                                  tr(ps[2 + k // 4][:, (k % 4) * 128:(k % 4 + 1) * 128], h2f[b_][:, k * 128:(k + 1) * 128], identf,
                                     [f"h2f{b_}", "cf"], [PK[2 + k // 4]])
                              cp("act", h2T[:, 0:4, :].rearrange("p k n -> p (k n)"), ps[2][:], [PK[2]], ["h2Ta"])
                              cp("dve", h2T[:, 4:8, :].rearrange("p k n -> p (k n)"), ps[3][:], [PK[3]], ["h2Tb"])
                              for k in range(8):
                                  mm(ps[4][:, 0:36], h2T[:, k, :], wr[:, k, :], ["h2Ta", "h2Tb", "wr"], [PK[4]], start=(k == 0), stop=(k == 7))
                              tt("dve", lg[:], ps[4][:, 0:36], rb[:], ALU.add, [PK[4], "rb"], ["lg"])
                              red(s5[:, 2:3], lg[:, 0:4], ALU.max, ["lg"], ["gmax"])
                              ts("dve", gm[:], lg[:, 0:4], s5[:, 2:3], None, ALU.is_equal, None, ["lg", "gmax"], ["gm"])
                              ts("dve", s5[:, 3:4], s5[:, 2:3], -1.0, None, ALU.mult, None, ["gmax"], ["ngmax"])
                              act(junk5[:, 0:4], lg[:, 0:4], AF.Exp, ["lg", "ngmax"], ["junk5", "gsum"], bias=s5[:, 3:4], accum_out=s5[:, 4:5])
                              recip(s5[:, 5:6], s5[:, 4:5], ["gsum"], ["gw"])
                              ts("dve", pen[:], gm[:], BIG, -BIG, ALU.mult, ALU.add, ["gm"], ["pen"])
                              tt("dve", lm[:].rearrange("p (g e) -> p g e", g=4), lg[:, 4:36].rearrange("p (g e) -> p g e", g=4),
                                 pen[:].unsqueeze(2).broadcast_to([128, 4, 8]), ALU.add, ["lg", "pen"], ["lm"])
                              red(s5[:, 6:7], lm[:], ALU.max, ["lm"], ["m1"])
                              ts("dve", m1all[:, t, :], lm[:], s5[:, 6:7], None, ALU.is_equal, None, ["lm", "m1"], [f"mk1_{t}"])
                              stt(lm2[:], m1all[:, t, :], -BIG, lm[:], ALU.mult, ALU.add, [f"mk1_{t}", "lm"], ["lm2"])
                              red(s5[:, 7:8], lm2[:], ALU.max, ["lm2"], ["m2"])
                              ts("dve", m2all[:, t, :], lm2[:], s5[:, 7:8], None, ALU.is_equal, None, ["lm2", "m2"], [f"mk2_{t}"])
                              tt("dve", s5[:, 8:9], s5[:, 7:8], s5[:, 6:7], ALU.subtract, ["m1", "m2"], ["dm"])
                              act(s5[:, 9:10], s5[:, 8:9], AF.Exp, ["dm"], ["edm"])
                              ts("dve", s5[:, 9:10], s5[:, 9:10], 1.0, None, ALU.add, None, ["edm"], ["edm"])
                              recip(s5[:, 10:11], s5[:, 9:10], ["edm"], ["p1"])
                              tt("dve", w12[:, t, 0:1], s5[:, 10:11], s5[:, 5:6], ALU.mult, ["p1", "gw"], [f"w1_{t}"])
                              tt("dve", w12[:, t, 1:2], s5[:, 5:6], w12[:, t, 0:1], ALU.subtract, ["gw", f"w1_{t}"], [f"w2_{t}"])
                              tt("dve", Mb[:], m1all[:, t, :], m2all[:, t, :], ALU.add, [f"mk1_{t}", f"mk2_{t}"], ["Mb"])
                              mm(ps[5][:, 0:32], Lstr, Mb[:], ["cb", "Mb"], [PK[5]])
                              mm(ps[5][:, 32:64], onesb, Mb[:], ["cb", "Mb"], [PK[5]])
                              tt("dve", rkall[:, t, :], ps[5][:, 0:32], base[:], ALU.add, [PK[5], "base"], [f"rk_{t}"])
                              tt("dve", base[:], ps[5][:, 32:64], base[:], ALU.add, [PK[5], "base"], ["base"])
                          for j in range(4):
                              t = c0 // 128 + j
                              b_ = tix % 2
                              tix += 1
                              g_ = p5gen(t, b_, j)
                              next(g_)
                              if prev5[0] is not None:
                                  for _ in prev5[0]:
                                      pass
                              prev5[0] = g_
                  if prev5[0] is not None:
                      for _ in prev5[0]:
                          pass
              S.barrier()
              with ExitStack() as cx:
                  cnt_ = sb(cx, "cnt_", [128, 32], F32)
                  pad_ = sb(cx, "pad_", [128, 32], F32)
                  pe_ = [sb(cx, f"pe_{i}", [128, 32], F32) for i in range(2)]
                  pst = sb(cx, "pst", [128, 32], F32)
                  cmpb = sb(cx, "cmpb", [128, NB, 32], F32)
                  bef = sb(cx, "bef", [128, NB], F32)
                  wif = sb(cx, "wif", [128, NB, 12], F32)
                  prk = sb(cx, "prk", [128, NT, 32], F32)
                  J = (2 * T) // BK
                  cmpj = sb(cx, "cmpj", [128, 32, J], F32)
                  tt("dve", cmpj[:], base[:].unsqueeze(2).broadcast_to([128, 32, J]), thr[:, 0:J].unsqueeze(1).broadcast_to([128, 32, J]),
                     ALU.is_gt, ["base", "cmoe"], ["cmpj"])
                  red(cnt_[:], cmpj[:], ALU.add, ["cmpj"], ["cnt_"])
                  ts("dve", pad_[:], cnt_[:], float(BK), None, ALU.mult, None, ["cnt_"], ["pad_"])
                  cp("dve", pe_[0][:], pad_[:], ["pad_"], ["pe_0"])
                  cur = 0
                  pk = ["pe_0"]
                  for sh in (1, 2, 4, 8, 16):
                      nx = 1 - cur
                      cp("dve", pe_[nx][:, 0:sh], pe_[cur][:, 0:sh], pk, [f"pe_{nx}a"])
                      tt("dve", pe_[nx][:, sh:32], pe_[cur][:, sh:32], pe_[cur][:, 0:32 - sh], ALU.add, pk, [f"pe_{nx}b"])
                      pk = [f"pe_{nx}a", f"pe_{nx}b"]
                      cur = nx
                  pend = pe_[cur]
                  tt("dve", pst[:], pend[:], pad_[:], ALU.subtract, pk + ["pad_"], ["pst"])
                  tt("dve", cmpb[:], pend[:].unsqueeze(1).broadcast_to([128, NB, 32]), thr.unsqueeze(2).broadcast_to([128, NB, 32]),
                     ALU.is_le, pk + ["cmoe"], ["cmpb"])
                  red(bef[:], cmpb[:], ALU.add, ["cmpb"], ["bef"])
                  ts("dve", bef[:], bef[:], 31.0, 32.0 * l, ALU.min, ALU.add, ["bef"], ["bef"])
                  tt("dve", wif[:], bef[:].unsqueeze(2).broadcast_to([128, NB, 12]), wstride.unsqueeze(1).broadcast_to([128, NB, 12]),
                     ALU.mult, ["bef", "cmoe"], ["wif"])
                  tt("dve", wif[:], wif[:], wcst.unsqueeze(1).broadcast_to([128, NB, 12]), ALU.add, ["wif", "cmoe"], ["wif"])
                  cp("dve", widx[:], wif[:], ["wif"], ["widx"])
                  allrk = [f"rk_{t}" for t in range(NT)]
                  allm1 = [f"mk1_{t}" for t in range(NT)]
                  allm2 = [f"mk2_{t}" for t in range(NT)]
                  tt("dve", prk[:], rkall[:], pst[:].unsqueeze(1).broadcast_to([128, NT, 32]), ALU.add, allrk + ["pst"], ["prk"])
                  tt("dve", rkall[:], prk[:], m1all[:], ALU.mult, ["prk"] + allm1 + allrk, ["rk1"])
                  red(destf[:, :, 0], rkall[:], ALU.add, ["rk1"], ["df0"])
                  tt("dve", rkall[:], prk[:], m2all[:], ALU.mult, ["prk", "rk1", "df0"] + allm2, ["rk2"])
                  red(destf[:, :, 1], rkall[:], ALU.add, ["rk2"], ["df1"])
                  cp("dve", desti[:], destf[:], ["df0", "df1"], ["desti"])
              S.barrier()
              stop5 = upto <= 5
              with ExitStack() as cx:
                  hb6 = [sb(cx, f"hb6{i}", [128, D], BF16) for i in range(4)]
                  for t in range(0 if stop5 else NT):
                      b_ = t % 4
                      dma("sp", hb6[b_][:], H2[t * 128:(t + 1) * 128, :], f"hb6{b_}", w=[f"hb6{b_}"])
                      scatter(HS[:, :], hb6[b_][:], desti[:, t, 0:1], f"sc{b_}", r=[f"hb6{b_}"])
                      scatter(HS[:, :], hb6[b_][:], desti[:, t, 1:2], f"sc{b_}", r=[f"hb6{b_}"])
              S.barrier()
              stop6 = upto <= 6
              with ExitStack() as cx:
                  w1b = [sb(cx, f"w1b{i}", [128, 8, 512], BF16) for i in range(2)]
                  w3b = [sb(cx, f"w3b{i}", [128, 8, 512], BF16) for i in range(2)]
                  w2b = [sb(cx, f"w2b{i}", [128, 4, D], BF16) for i in range(2)]
                  xs_ = [sb(cx, f"xs{i}", [128, 4, D], BF16) for i in range(2)]
                  xsT = sb(cx, "xsT", [128, 8, 512], BF16)
                  sgm = sb(cx, "sgm", [128, 512], F32)
                  hmT = sb(cx, "hmT", [128, 4, 512], BF16)
                  ybs = [sb(cx, f"ybs{i}", [128, 4, D], BF16) for i in range(2)]
                  w1rows = ew1.rearrange("l e (p a j) n -> (l e p a) (j n)", a=2, j=4)
                  w3rows = ew3.rearrange("l e (p a j) n -> (l e p a) (j n)", a=2, j=4)
                  w2rows = ew2.rearrange("l e (p a j) n -> (l e p a) (j n)", a=2, j=2)
                  for b in range(0 if (stop5 or stop6) else NB):
                      b_ = b % 2
                      for a_ in range(2):
                          gather(w1b[b_][:].rearrange("p k n -> p (k n)")[:, a_ * 2048:(a_ + 1) * 2048], w1rows, widx[:, b, a_:a_ + 1],
                                 f"gw{b_}", r=["widx"], w=[f"w1b{b_}_{a_}"])
                          gather(w3b[b_][:].rearrange("p k n -> p (k n)")[:, a_ * 2048:(a_ + 1) * 2048], w3rows, widx[:, b, a_:a_ + 1],
                                 f"gw{b_}", r=["widx"], w=[f"w3b{b_}_{a_}"])
                          gather(w2b[b_][:].rearrange("p k n -> p (k n)")[:, a_ * 2048:(a_ + 1) * 2048], w2rows, widx[:, b, a_:a_ + 1],
                                 f"gw{b_}", r=["widx"], w=[f"w2b{b_}_{a_}"])
                      dma("sp", xs_[b_][:], HS[b * BK:(b + 1) * BK, :].rearrange("(j p) n -> p j n", p=128), f"xs{b_}", w=[f"xs{b_}"])
                      for hk in range(2):
                          for kk in range(4):
                              k = hk * 4 + kk
                              tpv = ps[kk // 2][:].bitcast(BF16)
                              for j in range(4):
                                  tr(tpv[:, (kk % 2) * 512 + j * 128:(kk % 2) * 512 + (j + 1) * 128], xs_[b_][:, j, k:D:8],
                                     identb, [f"xs{b_}", "cb"], [PK[kk // 2]])
                          for q_ in range(2):
                              cp("act", xsT[:, hk * 4 + q_ * 2:hk * 4 + q_ * 2 + 2, :].rearrange("p k n -> p (k n)"),
                                 ps[q_][:].bitcast(BF16), [PK[q_]], [f"xsT{hk}{q_}"])
                      xk = [f"xsT{a}{c_}" for a in range(2) for c_ in range(2)]
                      for m in range(4):
                          pa = ps[2 + (m % 2) * 2]
                          pb2 = ps[3 + (m % 2) * 2]
                          ka, kb = PK[2 + (m % 2) * 2], PK[3 + (m % 2) * 2]
                          for k in range(8):
                              mm(pa[:], w1b[b_][:, k, m:512:4], xsT[:, k, :], [f"w1b{b_}_0", f"w1b{b_}_1"] + xk, [ka], start=(k == 0), stop=(k == 7))
                          for k in range(8):
                              mm(pb2[:], w3b[b_][:, k, m:512:4], xsT[:, k, :], [f"w3b{b_}_0", f"w3b{b_}_1"] + xk, [kb], start=(k == 0), stop=(k == 7))
                          act(sgm[:], pa[:], AF.Silu, [ka], ["sgm"])
                          tt("dve", hmT[:, m, :], sgm[:], pb2[:], ALU.mult, ["sgm", kb], [f"hmT{m}"])
                      hk_ = [f"hmT{m}" for m in range(4)]
                      for j in range(4):
                          for n in range(2):
                              pq = 6 + n
                              for m in range(4):
                                  mm(ps[pq][:], hmT[:, m, j * 128:(j + 1) * 128], w2b[b_][:, m, n * 512:(n + 1) * 512],
                                     hk_ + [f"w2b{b_}_0", f"w2b{b_}_1"], [PK[pq]], start=(m == 0), stop=(m == 3))
                              cp("act" if n == 0 else "dve", ybs[b_][:, j, n * 512:(n + 1) * 512], ps[pq][:], [PK[pq]], [f"ybs{b_}_{j}{n}"])
                      dma("sp", YB[b * BK:(b + 1) * BK, :].rearrange("(j p) n -> p j n", p=128), ybs[b_][:], f"ybst{b_}",
                          r=[f"ybs{b_}_{j}{n}" for j in range(4) for n in range(2)])
              S.barrier()
              stop7 = upto <= 7
              with ExitStack() as cx:
                  y1 = [sb(cx, f"y1_{i}", [128, D], BF16) for i in range(2)]
                  y2 = [sb(cx, f"y2_{i}", [128, D], BF16) for i in range(2)]
                  xt = [sb(cx, f"xt8{i}", [128, D], F32) for i in range(2)]
                  xo = [sb(cx, f"xo{i}", [128, D], F32) for i in range(2)]
                  t8 = [sb(cx, f"t8_{i}", [128, D], F32) for i in range(2)]
                  t9 = [sb(cx, f"t9_{i}", [128, D], F32) for i in range(2)]
                  g2b = [sb(cx, f"g2b{i}", [128, D], F32) for i in range(2)]
                  for si, Ss in enumerate([] if (stop5 or stop6 or stop7) else seqs):
                      mb = si % 2
                      dma("sp", g2b[mb][:], MOD[si:si + 1, 5 * D:6 * D].partition_broadcast(128), f"g2{mb}", w=[f"g2b{mb}"])
                      for tl in range(Ss // 128):
                          t = soff[si] // 128 + tl
                          b_ = t % 2
                          gather(y1[b_][:], YB[:, :], desti[:, t, 0:1], f"gy{b_}", r=["desti"], w=[f"y1_{b_}"])
                          gather(y2[b_][:], YB[:, :], desti[:, t, 1:2], f"gy{b_}", r=["desti"], w=[f"y2_{b_}"])
                          dma("sp", xt[b_][:], XM[t * 128:(t + 1) * 128, :], f"xt8{b_}", w=[f"xt8{b_}"])
                          act(t8[b_][:], y1[b_][:], AF.Copy, [f"y1_{b_}"], [f"t8{b_}"], scale=w12[:, t, 0:1])
                          stt(t9[b_][:], y2[b_][:], w12[:, t, 1:2], t8[b_][:], ALU.mult, ALU.add, [f"y2_{b_}", f"t8{b_}"], [f"t9{b_}"])
                          tt("dve", t9[b_][:], t9[b_][:], g2b[mb][:], ALU.mult, [f"t9{b_}", f"g2b{mb}"], [f"t9{b_}"])
                          tt("dve", xo[b_][:], t9[b_][:], xt[b_][:], ALU.add, [f"t9{b_}", f"xt8{b_}"], [f"xo{b_}"])
                          dma("sp", x_dst[t * 128:(t + 1) * 128, :], xo[b_][:], f"xos{b_}", r=[f"xo{b_}"])
              S.barrier()
          x_src = XR

    except _Stop:
        pass
    nops = S.emit()
    return nc, stack, nops, NB


def host_consts(seqs):
    T = sum(seqs)
    NB = (2 * T) // BK + NEXP
    Smax = max(seqs)
    bf = ml_dtypes.bfloat16
    p = np.arange(128)
    cf = np.zeros((128, 1792), np.float32)
    cf[:, 0:128] = np.eye(128)
    cf[:, 128:256] = (p[:, None] <= p[None, :])
    cf[:, 256:384] = (p[:, None] >= p[None, :])
    cf[:, 384:512] = 1.0
    mf = (p[:, None] <= p[None, :]).astype(np.float32)
    mb = (p[:, None] > p[None, :]).astype(np.float32)
    cf[:, 512:1024] = np.tile(mf, (1, 4))
    cf[:, 1024:1536] = np.tile(mb, (1, 4))
    cf[64, 1536:1600] = 1.0
    cb = np.zeros((128, 1408), np.float32)
    cb[:, 0:128] = np.eye(128)
    cb[:, 128:256] = 1.0
    cb[:, 256:384] = (p[:, None] < p[None, :])
    hm = np.zeros((128, 4, 128), np.float32)
    for h in range(4):
        hm[32 * h:32 * (h + 1), h, :] = 1.0
    cb[:, 384:896] = hm.reshape(128, 512)
    hc = np.zeros((128, 4, 128), np.float32)
    for h in range(4):
        hc[:, h, 32 * h:32 * (h + 1)] = 1.0
    cb[:, 896:1408] = hc.reshape(128, 512)
    half = 16
    freqs = 10000.0 ** (-np.arange(half, dtype=np.float32) / half)
    pos = np.arange(Smax, dtype=np.float32)
    ang = pos[:, None] * freqs[None, :]
    rope = np.concatenate([np.cos(ang), np.sin(ang)], axis=1).astype(np.float32)
    rope = rope.reshape(Smax // 128, 128, 32).transpose(1, 0, 2).copy()
    cm = np.zeros((128, 32 + NB + 24), np.float32)
    cm[:, 0:32] = np.arange(32)[None, :]
    cm[:, 32:32 + NB] = (np.arange(NB) * BK)[None, :]
    cm[:, 32 + NB:32 + NB + 8] = 256.0
    cm[:, 32 + NB + 8:32 + NB + 12] = 512.0
    cm[:, 32 + NB + 12:32 + NB + 24] = 2 * p[:, None]
    cm[:, 32 + NB + 13] += 1
    c64 = np.arange(64)
    a64 = 2 * np.pi * np.outer(c64, c64) / 64.0
    Cc = np.cos(a64) / 8.0
    Sc = np.sin(a64) / 8.0
    cch = np.zeros((128, 256), np.float32)
    for g in range(2):
        cch[64 * g:64 * (g + 1), 64 * g:64 * (g + 1)] = Cc
        cch[64 * g:64 * (g + 1), 128 + 64 * g:128 + 64 * (g + 1)] = -Sc
    out = {"c_f32": cf, "c_bf": cb.astype(bf), "c_rope": rope, "c_moe": cm, "c_chan": cch.astype(bf)}
    for s_ in sorted(set(seqs)):
        n = np.arange(s_, dtype=np.int64)
        m = np.outer(n, n) % s_
        a = 2 * np.pi * m.astype(np.float64) / s_
        sc = 1.0 / np.sqrt(s_)
        out[f"dftc{s_}"] = (np.cos(a) * sc).astype(np.float32).astype(bf)
        out[f"dfts{s_}"] = (np.sin(a) * sc).astype(np.float32).astype(bf)
    return out


WNAMES = ["ada_w", "ada_b", "norm1_g", "norm2_g", "w_in", "w_out", "gla_gate_up_f", "gla_gate_bias_f", "gla_gate_up_b",
          "gla_gate_bias_b", "gla_out_norm_g", "mla_q_lora_norm_g", "mla_w_uq", "mla_kv_lora_norm_g", "mla_w_ukv",
          "mla_q_norm_g", "mla_k_norm_g", "router_group_w", "router_group_b", "router_expert_w", "router_expert_b",
          "expert_w1", "expert_w3", "expert_w2"]


def run(xs_per_core, cs_per_core, weights, seqs, depth, debug=False):
    nc, stack, nops, NB = build(seqs, depth, debug=debug)
    consts = host_consts(seqs)
    in_maps = []
    for xc, cc in zip(xs_per_core, cs_per_core):
        m = {"x_all": xc, "cT": cc}
        for k in WNAMES:
            m[k] = weights[k]
        m.update(consts)
        in_maps.append(m)
    res = run_bass_kernel_spmd(nc, in_maps, core_ids=list(range(len(in_maps))))
    stack.close()
    return res


def kernel(x_prompt, x_sample, c_prompt, c_sample, **weights):
    ncore = 8
    xp = np.asarray(x_prompt, np.float32)
    xs = np.asarray(x_sample, np.float32)
    cp_ = np.asarray(c_prompt, np.float32)
    cs_ = np.asarray(c_sample, np.float32)
    Bp, Sp, _ = xp.shape
    Bs, Ss, _ = xs.shape
    npc, nsc = Bp // ncore, Bs // ncore
    seqs = [Sp] * npc + [Ss] * nsc
    depth = np.asarray(weights["ada_w"]).shape[0]
    w = {k: np.ascontiguousarray(np.asarray(weights[k], np.float32)) for k in WNAMES}
    xcs, ccs = [], []
    for i in range(ncore):
        xcs.append(np.ascontiguousarray(np.concatenate(
            [xp[i * npc:(i + 1) * npc].reshape(-1, D), xs[i * nsc:(i + 1) * nsc].reshape(-1, D)], axis=0)))
        ccs.append(np.ascontiguousarray(np.concatenate([cp_[i * npc:(i + 1) * npc], cs_[i * nsc:(i + 1) * nsc]], axis=0).T))
    res = run(xcs, ccs, w, seqs, depth)
    yp = np.zeros_like(xp)
    ys = np.zeros_like(xs)
    for i in range(ncore):
        y = res.results[i]["y_all"]
        yp[i * npc:(i + 1) * npc] = y[:npc * Sp].reshape(npc, Sp, D)
        ys[i * nsc:(i + 1) * nsc] = y[npc * Sp:].reshape(nsc, Ss, D)
    return (yp, ys)
```

```python
from contextlib import ExitStack
import os
import numpy as np
import ml_dtypes
import concourse.bass as bass
import concourse.mybir as mybir
from concourse.bass_utils import run_bass_kernel_spmd

F32 = mybir.dt.float32
BF16 = mybir.dt.bfloat16
I32 = mybir.dt.int32
ALU = mybir.AluOpType
AF = mybir.ActivationFunctionType
AX = mybir.AxisListType

D = 1024
PU = 1696
EPS = 1e-6
NEXP = 32
BK = 512
BIG = 1.0e4


class Sch:
    def __init__(self, nc, stack):
        self.nc = nc
        self.stack = stack
        self.ops = []
        self.keys = {}
        self.lastop = {}
        self.pending = {e: {} for e in ("pe", "act", "dve", "pool", "sp")}
        self.eng = {"pe": nc.tensor, "act": nc.scalar, "dve": nc.vector, "pool": nc.gpsimd, "sp": nc.sync}

    def op(self, eng, fn, r=(), w=(), chan=None):
        deps = {}

        def add(tok):
            for s_, o_ in tok.items():
                if s_ not in self.eng:
                    o_ = self.lastop[s_]
                if deps.get(s_, -1) < o_:
                    deps[s_] = o_

        for k in r:
            st = self.keys.get(k)
            if st:
                add(st[0])
        for k in w:
            st = self.keys.get(k)
            if st:
                add(st[0])
                add(st[1])
        add(self.pending[eng])
        self.pending[eng] = {}
        src = chan if chan else eng
        oid = len(self.ops)
        self.ops.append((eng, fn, deps, src))
        for k in w:
            self.keys[k] = [{src: oid}, {}]
        for k in r:
            st = self.keys.setdefault(k, [{}, {}])
            st[1][src] = oid
        self.lastop[src] = oid

    def barrier(self):
        for e in self.pending:
            self.pending[e] = dict(self.lastop)
        self.keys.clear()

    def emit(self):
        nc = self.nc
        need = set()
        for (_, _, deps, _) in self.ops:
            need.update(deps.values())
        sems = {}

        def sem(src):
            if src not in sems:
                sems[src] = self.stack.enter_context(nc.semaphore("s_" + str(src)))
            return sems[src]

        semval = {}
        cnt = {}
        known = {e: {} for e in self.eng}
        for oid, (eng, fn, deps, src) in enumerate(self.ops):
            E = self.eng[eng]
            for dsrc, doid in deps.items():
                if dsrc == "pe" and eng == "pe":
                    continue
                v = semval[doid]
                if known[eng].get(dsrc, 0) < v:
                    E.wait_ge(sem(dsrc), v)
                    known[eng][dsrc] = v
            ins = fn(E)
            if src != eng:
                cnt[src] = cnt.get(src, 0) + 16
                ins.then_inc(sem(src), 16)
                semval[oid] = cnt[src]
            elif oid in need:
                cnt[src] = cnt.get(src, 0) + 1
                ins.then_inc(sem(src), 1)
                semval[oid] = cnt[src]
        for src, c in cnt.items():
            nc.sync.wait_ge(sem(src), c)
        return len(self.ops)


class _Stop(Exception):
    pass


def build(seqs, depth, debug=False, upto=99):
    nseq = len(seqs)
    T = sum(seqs)
    NT = T // 128
    NB = (2 * T) // BK + NEXP
    soff = [sum(seqs[:i]) for i in range(nseq)]
    Smax = max(seqs)
    Sset = sorted(set(seqs))

    nc = bass.Bass("TRN2", target_bir_lowering=False)
    stack = ExitStack()
    S = Sch(nc, stack)

    def din(name, shape, dt=F32):
        return nc.dram_tensor(name, list(shape), dt, kind="ExternalInput").ap()

    def dscr(name, shape, dt):
        if debug:
            return nc.dram_tensor(name, list(shape), dt, kind="ExternalOutput").ap()
        return nc.dram_tensor(name, list(shape), dt).ap()

    x_in = din("x_all", [T, D])
    cT_in = din("cT", [D, nseq])
    ada_w = din("ada_w", [depth, D, 6 * D])
    ada_b = din("ada_b", [depth, 6 * D])
    norm1_g = din("norm1_g", [depth, D])
    norm2_g = din("norm2_g", [depth, D])
    w_in = din("w_in", [depth, D, 1472])
    w_out = din("w_out", [depth, D, D])
    up_f = din("gla_gate_up_f", [depth, 16, 128])
    bias_f = din("gla_gate_bias_f", [depth, 128])
    up_b = din("gla_gate_up_b", [depth, 16, 128])
    bias_b = din("gla_gate_bias_b", [depth, 128])
    gla_ng = din("gla_out_norm_g", [depth, 64])
    qlora_g = din("mla_q_lora_norm_g", [depth, 256])
    w_uq = din("mla_w_uq", [depth, 256, 768])
    kvlora_g = din("mla_kv_lora_norm_g", [depth, 128])
    w_ukv = din("mla_w_ukv", [depth, 128, 1024])
    qn_g = din("mla_q_norm_g", [depth, 96])
    kn_g = din("mla_k_norm_g", [depth, 96])
    rg_w = din("router_group_w", [depth, D, 4])
    rg_b = din("router_group_b", [depth, 4])
    re_w = din("router_expert_w", [depth, D, 32])
    re_b = din("router_expert_b", [depth, 32])
    ew1 = din("expert_w1", [depth, 32, D, 512])
    ew3 = din("expert_w3", [depth, 32, D, 512])
    ew2 = din("expert_w2", [depth, 32, 512, D])
    c_f32 = din("c_f32", [128, 1792])
    c_bf = din("c_bf", [128, 1408], BF16)
    c_rope = din("c_rope", [128, Smax // 128, 32])
    c_moe = din("c_moe", [128, 32 + NB + 12 + 12])
    c_chan = din("c_chan", [128, 256], BF16)
    dft = {s_: (din(f"dftc{s_}", [s_, s_], BF16), din(f"dfts{s_}", [s_, s_], BF16)) for s_ in Sset}
    y_out = nc.dram_tensor("y_all", [T, D], F32, kind="ExternalOutput").ap()
    MOD = dscr("MOD", [nseq, 6 * D], F32)
    U = dscr("U", [T, PU], BF16)
    MIXT = dscr("MIXT", [D, T], BF16)
    XM = dscr("XM", [T, D], F32)
    XR = dscr("XR", [T, D], F32)
    H2 = dscr("H2", [T, D], BF16)
    HS = dscr("HS", [NB * BK, D], BF16)
    YB = dscr("YB", [NB * BK, D], BF16)

    uniq = [0]

    def sb(ctx, name, shape, dt):
        uniq[0] += 1
        return ctx.enter_context(nc.sbuf_tensor(f"{name}_{uniq[0]}", list(shape), dt))

    ps = [stack.enter_context(nc.psum_tensor(f"ps{i}", [128, 512], F32)) for i in range(8)]
    PK = [f"ps{i}" for i in range(8)]

    def mm(out, lhsT, rhs, r, w, start=True, stop=True):
        S.op("pe", lambda E: E.matmul(out, lhsT=lhsT, rhs=rhs, start=start, stop=stop), r, w)

    def tr(out, in_, ident, r, w):
        S.op("pe", lambda E: E.transpose(out, in_, ident), r, w)

    def act(out, in_, func, r, w, bias=None, scale=None, accum_out=None):
        kw = {}
        if bias is not None:
            kw["bias"] = bias
        if scale is not None:
            kw["scale"] = scale
        if accum_out is not None:
            kw["accum_out"] = accum_out
        S.op("act", lambda E: E.activation(out, in_, func, **kw), r, w)

    def tt(eng, out, in0, in1, op, r, w):
        S.op(eng, lambda E: E.tensor_tensor(out, in0, in1, op), r, w)

    def ts(eng, out, in0, s1, s2, op0, op1, r, w):
        if op1 is None:
            S.op(eng, lambda E: E.tensor_scalar(out, in0, s1, None, op0), r, w)
        else:
            S.op(eng, lambda E: E.tensor_scalar(out, in0, s1, s2, op0, op1), r, w)

    def stt(out, in0, sc, in1, op0, op1, r, w):
        S.op("dve", lambda E: E.scalar_tensor_tensor(out, in0, sc, in1, op0, op1), r, w)

    def cp(eng, out, in_, r, w):
        if eng == "act":
            S.op("act", lambda E: E.copy(out, in_), r, w)
        else:
            S.op(eng, lambda E: E.tensor_copy(out, in_), r, w)

    def red(out, in_, op, r, w, axis=AX.X):
        S.op("dve", lambda E: E.tensor_reduce(out, in_, axis, op), r, w)

    def recip(out, in_, r, w):
        S.op("dve", lambda E: E.reciprocal(out, in_), r, w)

    def memset(eng, ap, val, w):
        S.op(eng, lambda E: E.memset(ap, val), (), w)

    def dma(q, out, in_, chan, r=(), w=()):
        S.op(q, lambda E: E.dma_start(out=out, in_=in_), r, w, chan=chan)

    def gather(out, in_, idx, chan, r=(), w=()):
        S.op("pool", lambda E: E.indirect_dma_start(out=out, out_offset=None, in_=in_,
                                                     in_offset=bass.IndirectOffsetOnAxis(ap=idx, axis=0)), r, w, chan=chan)

    def scatter(out, in_, idx, chan, r=(), w=()):
        S.op("pool", lambda E: E.indirect_dma_start(out=out, out_offset=bass.IndirectOffsetOnAxis(ap=idx, axis=0),
                                                     in_=in_, in_offset=None), r, w, chan=chan)

    def rstd_of(out, ss, n, r, w):
        act(out, ss, AF.Ln, r, w, scale=1.0 / n, bias=EPS)
        act(out, out, AF.Exp, w, w, scale=-0.5)

    cf = sb(stack, "cf", [128, 1792], F32)
    cb = sb(stack, "cb", [128, 1408], BF16)
    crope = sb(stack, "crope", [128, Smax // 128, 32], F32)
    cmoe = sb(stack, "cmoe", [128, 32 + NB + 24], F32)
    cchan = sb(stack, "cchan", [128, 256], BF16)
    dma("sp", cf[:], c_f32[:, :], "c0", w=["cf"])
    dma("sp", cb[:], c_bf[:, :], "c0", w=["cb"])
    dma("sp", crope[:], c_rope[:, :, :], "c0", w=["crope"])
    dma("sp", cmoe[:], c_moe[:, :], "c0", w=["cmoe"])
    dma("sp", cchan[:], c_chan[:, :], "c0", w=["cchan"])
    identf = cf[:, 0:128]
    Uincl = cf[:, 128:256]
    Urev = cf[:, 256:384]
    onesf = cf[:, 384:512]
    maskf4 = cf[:, 512:1024]
    maskb4 = cf[:, 1024:1536]
    sel65 = cf[0:65, 1536:1600]
    identb = cb[:, 0:128]
    onesb = cb[:, 128:256]
    Lstr = cb[:, 256:384]
    hm4 = cb[:, 384:896]
    hmcol = cb[:, 896:1408]
    CK = ["cf", "cb", "crope", "cmoe", "cchan"]

    cact = sb(stack, "cact", [128, 8, nseq], F32)
    dma("sp", cact[:], cT_in.rearrange("(k p) n -> p k n", p=128), "c0", w=["cact"])
    act(cact[:], cact[:], AF.Silu, ["cact"], ["cact"])
    S.barrier()

    x_src = x_in
    try:
      for l in range(depth):
          x_dst = y_out if l == depth - 1 else XR
          with ExitStack() as cx:
              awb = [sb(cx, f"awb{i}", [128, 8, 512], F32) for i in range(2)]
              modsb = sb(cx, "modsb", [nseq, 6 * D], F32)
              adab = sb(cx, "adab", [nseq, 6 * D], F32)
              g12 = sb(cx, "g12", [nseq, 2 * D], F32)
              dma("sp", adab[:], ada_b[l:l + 1, :].partition_broadcast(nseq), "p0m", w=["adab"])
              dma("sp", g12[:, 0:D], norm1_g[l:l + 1, :].partition_broadcast(nseq), "p0m", w=["g12a"])
              dma("sp", g12[:, D:2 * D], norm2_g[l:l + 1, :].partition_broadcast(nseq), "p0m", w=["g12b"])
              for n in range(12):
                  b_ = n % 2
                  dma("sp", awb[b_][:], ada_w[l].rearrange("(k p) n -> p k n", p=128)[:, :, n * 512:(n + 1) * 512],
                      f"aw{b_}", w=[f"awb{b_}"])
                  pb = PK[n % 2]
                  for k in range(8):
                      mm(ps[n % 2][0:nseq, :], cact[:, k, :], awb[b_][:, k, :], ["cact", f"awb{b_}"], [pb],
                         start=(k == 0), stop=(k == 7))
                  tt("dve", modsb[:, n * 512:(n + 1) * 512], ps[n % 2][0:nseq, :], adab[:, n * 512:(n + 1) * 512],
                     ALU.add, [pb, "adab"], [f"mod{n}"])
              allmod = [f"mod{n}" for n in range(12)]
              stt(modsb[:, D:2 * D], modsb[:, D:2 * D], 1.0, g12[:, 0:D], ALU.add, ALU.mult, allmod + ["g12a"], ["mod2", "mod3"])
              stt(modsb[:, 4 * D:5 * D], modsb[:, 4 * D:5 * D], 1.0, g12[:, D:2 * D], ALU.add, ALU.mult,
                  allmod + ["g12b", "mod2", "mod3"], ["mod8", "mod9"])
              dma("sp", MOD[:, :], modsb[:], "p0s", r=allmod)
          S.barrier()

          if upto <= 0:
              raise _Stop()
          with ExitStack() as cx:
              weff = sb(cx, "weff", [128, 8, PU], BF16)
              with ExitStack() as cx2:
                  wst = sb(cx2, "wst", [128, 4, 1472], F32)
                  gfT = sb(cx2, "gfT", [16, 2, 8, 128], F32)
                  upfb = sb(cx2, "upfb", [16, 2, 128], F32)
                  dma("sp", upfb[:, 0, :], up_f[l], "p1w", w=["upf"])
                  dma("sp", upfb[:, 1, :], up_b[l], "p1w", w=["upb"])
                  for half in range(2):
                      dma("sp", wst[:], w_in[l].rearrange("(k p) n -> p k n", p=128)[:, half * 4:(half + 1) * 4, :],
                          "p1w", w=["wst"])
                      ks = slice(half * 4, half * 4 + 4)
                      cp("dve", weff[:, ks, 0:768], wst[:, :, 0:768], ["wst"], [f"weffA{half}"])
                      cp("pool", weff[:, ks, 1024:PU], wst[:, :, 800:1472], ["wst"], [f"weffB{half}"])
                      for kk in range(4):
                          for d_ in range(2):
                              tr(ps[2 * d_][0:16, kk * 128:(kk + 1) * 128],
                                 wst[:, kk, 768 + 16 * d_:784 + 16 * d_], identf, ["wst", "cf"], [PK[2 * d_]])
                      for d_ in range(2):
                          cp("dve", gfT[:, d_, half * 4:half * 4 + 4, :].rearrange("p k n -> p (k n)"), ps[2 * d_][0:16, :],
                             [PK[2 * d_]], [f"gfT{half}{d_}"])
                      for kk in range(4):
                          k = half * 4 + kk
                          for d_ in range(2):
                              mm(ps[1 + 2 * d_][:, kk * 128:(kk + 1) * 128], gfT[:, d_, k, :], upfb[:, d_, :],
                                 [f"gfT{half}{d_}", "upf", "upb"], [PK[1 + 2 * d_]])
                      for d_ in range(2):
                          cp("dve", weff[:, half * 4:half * 4 + 4, 768 + 128 * d_:896 + 128 * d_],
                             ps[1 + 2 * d_][:].rearrange("p (k n) -> p k n", k=4), [PK[1 + 2 * d_]], [f"weffC{half}{d_}"])
                  S.barrier()
              xt = [sb(cx, f"xt{i}", [128, D], F32) for i in range(2)]
              hb = [sb(cx, f"hb{i}", [128, D], BF16) for i in range(2)]
              hT = [sb(cx, f"hT{i}", [128, 8, 128], BF16) for i in range(2)]
              ub = [sb(cx, f"ub{i}", [128, PU], BF16) for i in range(2)]
              tmpf = sb(cx, "tmpf", [128, D], F32)
              junk = sb(cx, "junk", [128, D], BF16)
              ssq = sb(cx, "ssq", [128, 4], F32)
              modb = [sb(cx, f"modb{i}", [128, 2 * D], F32) for i in range(2)]
              tix = 0
              prev1 = [None]
              for si, Ss in enumerate(seqs):
                  mb = si % 2
                  dma("sp", modb[mb][:], MOD[si:si + 1, 0:2 * D].partition_broadcast(128), f"md{mb}", w=[f"modb{mb}"])
                  def p1gen(t, b_, mb=mb):
                      dma("sp", xt[b_][:], x_src[t * 128:(t + 1) * 128, :], f"xt{b_}", w=[f"xt{b_}"])
                      act(junk[:], xt[b_][:], AF.Square, [f"xt{b_}"], ["junk", f"ssq{b_}"], accum_out=ssq[:, b_:b_ + 1])
                      rstd_of(ssq[:, 2 + b_:3 + b_], ssq[:, b_:b_ + 1], D, [f"ssq{b_}"], [f"rs{b_}"])
                      stt(tmpf[:], xt[b_][:], ssq[:, 2 + b_:3 + b_], modb[mb][:, D:2 * D], ALU.mult, ALU.mult,
                          [f"xt{b_}", f"rs{b_}", f"modb{mb}"], ["tmpf"])
                      tt("pool", hb[b_][:], tmpf[:], modb[mb][:, 0:D], ALU.add, ["tmpf", f"modb{mb}"], [f"hb{b_}"])
                      tpv = ps[4 + b_][:].bitcast(BF16)
                      for k in range(8):
                          tr(tpv[:, k * 128:(k + 1) * 128], hb[b_][:, k * 128:(k + 1) * 128], identb, [f"hb{b_}", "cb"], [PK[4 + b_]])
                      cp("act", hT[b_][:].rearrange("p k n -> p (k n)"), tpv, [PK[4 + b_]], [f"hT{b_}"])
                      yield
                      for n in range(4):
                          c0, c1 = n * 512, min(PU, (n + 1) * 512)
                          for k in range(8):
                              mm(ps[n][:, 0:c1 - c0], hT[b_][:, k, :], weff[:, k, c0:c1], [f"hT{b_}"], [PK[n]],
                                 start=(k == 0), stop=(k == 7))
                          cp("act" if n % 2 == 0 else "dve", ub[b_][:, c0:c1], ps[n][:, 0:c1 - c0], [PK[n]], [f"ub{b_}_{n}"])
                      dma("sp", U[t * 128:(t + 1) * 128, :], ub[b_][:], f"ust{b_}", r=[f"ub{b_}_{n}" for n in range(4)])
                  for tl in range(Ss // 128):
                      t = soff[si] // 128 + tl
                      b_ = tix % 2
                      tix += 1
                      g_ = p1gen(t, b_)
                      next(g_)
                      if prev1[0] is not None:
                          for _ in prev1[0]:
                              pass
                      prev1[0] = g_
              if prev1[0] is not None:
                  for _ in prev1[0]:
                      pass
          S.barrier()

          if upto <= 1:
              raise _Stop()
          with ExitStack() as cx:
              NCm = Smax // 128
              qkT = sb(cx, "qkT", [128, NCm, 512], BF16)
              vall = sb(cx, "vall", [128, NCm, 256], BF16)
              ogall = sb(cx, "ogall", [128, NCm, 256], BF16)
              dsall = sb(cx, "dsall", [128, NCm, 128], F32)
              decall = sb(cx, "decall", [128, NCm, 2], F32)
              stf = sb(cx, "stf", [128, NCm + 1, 128], F32)
              stb16 = sb(cx, "stb16", [128, NCm, 128], BF16)
              ug = [sb(cx, f"ug{i}", [128, 1024], BF16) for i in range(2)]
              gbias = sb(cx, "gbias", [128, 256], F32)
              gng = sb(cx, "gng", [128, 256], F32)
              zt = sb(cx, "zt", [128, 256], F32)
              gt = sb(cx, "gt", [128, 256], F32)
              cs = sb(cx, "cs", [128, 512], F32)
              dd = sb(cx, "dd", [128, 256], F32)
              epos = sb(cx, "epos", [128, 256], F32)
              eneg = sb(cx, "eneg", [128, 256], F32)
              eend = sb(cx, "eend", [128, 256], F32)
              qd = sb(cx, "qd", [128, 4, 128], BF16)
              kef = sb(cx, "kef", [128, 2, 128], F32)
              ke4 = sb(cx, "ke4", [128, 2, 4, 128], BF16)
              m4 = sb(cx, "m4", [128, 4, 4, 128], BF16)
              attm = sb(cx, "attm", [128, 8, 128], BF16)
              osq = sb(cx, "osq", [128, 256], F32)
              o1 = sb(cx, "o1", [128, 256], F32)
              sg = sb(cx, "sg", [128, 256], F32)
              oss = sb(cx, "oss", [128, 8], F32)
              ogl = sb(cx, "ogl", [128, 256], BF16)
              oT = [sb(cx, f"oT{i}", [128, 2, 128], BF16) for i in range(2)]
              dma("sp", gbias[:, 0:128], bias_f[l:l + 1, :].partition_broadcast(128), "p2c", w=["gbias"])
              dma("sp", gbias[:, 128:256], bias_b[l:l + 1, :].partition_broadcast(128), "p2c", w=["gbias"])
              for h in range(4):
                  dma("sp", gng[:, h * 64:(h + 1) * 64], gla_ng[l:l + 1, :].partition_broadcast(128), "p2c", w=["gng"])
              it = 0
              for si, Ss in enumerate(seqs):
                  NCs = Ss // 128
                  t0 = soff[si] // 128
                  for c in range(NCs):
                      t = t0 + c
                      b_ = it % 2
                      it += 1
                      dma("sp", ug[b_][:], U[t * 128:(t + 1) * 128, 0:1024], f"ug{b_}", w=[f"ug{b_}"])
                      cp("pool", vall[:, c, :], ug[b_][:, 256:512], [f"ug{b_}"], [f"v{c}"])
                      cp("pool", ogall[:, c, :], ug[b_][:, 512:768], [f"ug{b_}"], [f"og{c}"])
                      tt("dve", zt[:], ug[b_][:, 768:1024], gbias[:], ALU.add, [f"ug{b_}", "gbias"], ["zt"])
                      act(zt[:], zt[:], AF.Exp, ["zt"], ["zt"], scale=-1.0)
                      act(zt[:], zt[:], AF.Ln, ["zt"], ["zt"], bias=1.0)
                      ts("dve", gt[:], zt[:], -1.0 / 16.0, None, ALU.mult, None, ["zt"], ["gt"])
                      mm(ps[0][:, 0:128], Uincl, gt[:, 0:128], ["gt", "cf"], [PK[0]])
                      mm(ps[0][:, 128:256], Urev, gt[:, 128:256], ["gt", "cf"], [PK[0]])
                      mm(ps[0][:, 256:512], onesf, gt[:], ["gt", "cf"], [PK[0]])
                      mm(ps[1][:, 0:1], gt[:, 0:128], onesf[:, 0:1], ["gt", "cf"], [PK[1]])
                      mm(ps[1][:, 1:2], gt[:, 128:256], onesf[:, 0:1], ["gt", "cf"], [PK[1]])
                      cp("dve", cs[:], ps[0][:], [PK[0]], ["cs"])
                      act(decall[:, c, :], ps[1][:, 0:2], AF.Exp, [PK[1]], [f"dec{c}"])
                      tt("dve", dd[:], cs[:, 256:512], cs[:, 0:256], ALU.subtract, ["cs"], ["dd"])
                      act(epos[:], cs[:, 0:256], AF.Exp, ["cs"], ["epos"])
                      act(eneg[:], cs[:, 0:256], AF.Exp, ["cs"], ["eneg"], scale=-1.0)
                      act(eend[:], dd[:], AF.Exp, ["dd"], ["eend"])
                      qv = ug[b_][:, 0:128]
                      kv_ = ug[b_][:, 128:256]
                      for d_ in range(2):
                          stt(qd[:, 2 * d_, :], qv, 32.0 ** -0.5, epos[:, d_ * 128:(d_ + 1) * 128], ALU.mult, ALU.mult,
                              [f"ug{b_}", "epos"], [f"qd{2 * d_}"])
                          tt("dve", qd[:, 2 * d_ + 1, :], kv_, eneg[:, d_ * 128:(d_ + 1) * 128], ALU.mult,
                             [f"ug{b_}", "eneg"], [f"qd{2 * d_ + 1}"])
                          tt("dve", kef[:, d_, :], kv_, eend[:, d_ * 128:(d_ + 1) * 128], ALU.mult, [f"ug{b_}", "eend"], [f"kef{d_}"])
                          tt("dve", ke4[:, d_, :, :], kef[:, d_:d_ + 1, :].broadcast_to([128, 4, 128]),
                             hmcol.rearrange("p (h n) -> p h n", h=4),
                             ALU.mult, [f"kef{d_}", "cb"], [f"ke4{d_}"])
                      tpv = ps[2][:].bitcast(BF16)
                      for j in range(4):
                          tr(tpv[:, j * 128:(j + 1) * 128], qd[:, j, :], identb, [f"qd{j}", "cb"], [PK[2]])
                      cp("act", qkT[:, c, :], tpv[:, 0:512], [PK[2]], [f"qkT{c}"])
                      for d_ in range(2):
                          for h in range(4):
                              mm(ps[3][:, d_ * 64:(d_ + 1) * 64], ke4[:, d_, h, :], ug[b_][:, 256 + h * 64:256 + (h + 1) * 64],
                                 [f"ke4{d_}", f"ug{b_}"], [PK[3]], start=(h == 0), stop=(h == 3))
                      cp("dve", dsall[:, c, :], ps[3][:, 0:128], [PK[3]], [f"ds{c}"])
                  memset("dve", stf[:, 0, 0:64], 0.0, ["st_f0"])
                  for c in range(NCs):
                      stt(stf[:, c + 1, 0:64], stf[:, c, 0:64], decall[:, c, 0:1], dsall[:, c, 0:64], ALU.mult, ALU.add,
                          [f"st_f{c}", f"dec{c}", f"ds{c}"], [f"st_f{c + 1}"])
                  memset("dve", stf[:, NCs - 1, 64:128], 0.0, [f"st_b{NCs - 1}"])
                  for c in range(NCs - 1, 0, -1):
                      stt(stf[:, c - 1, 64:128], stf[:, c, 64:128], decall[:, c, 1:2], dsall[:, c, 64:128], ALU.mult, ALU.add,
                          [f"st_b{c}", f"dec{c}", f"ds{c}"], [f"st_b{c - 1}"])
                  allst = [f"st_f{c}" for c in range(NCs + 1)] + [f"st_b{c}" for c in range(NCs)]
                  cp("dve", stb16[:, 0:NCs, :], stf[:, 0:NCs, :], allst, ["stb16"])
                  for c in range(NCs):
                      t = t0 + c
                      for j, src in enumerate((1, 3, 0, 2)):
                          tt("dve" if j % 2 == 0 else "pool", m4[:, j, :, :],
                             qkT[:, c, src * 128:(src + 1) * 128].unsqueeze(1).broadcast_to([128, 4, 128]),
                             hm4.rearrange("p (h n) -> p h n", h=4), ALU.mult, [f"qkT{c}", "cb"], [f"m4{j}"])
                      for d_ in range(2):
                          for h in range(4):
                              mm(ps[4 + d_][:, h * 128:(h + 1) * 128], m4[:, d_, h, :], qkT[:, c, (2 * d_) * 128:(2 * d_ + 1) * 128],
                                 [f"m4{d_}", f"qkT{c}"], [PK[4 + d_]])
                          tt("dve", attm[:, d_ * 4:(d_ + 1) * 4, :].rearrange("p h n -> p (h n)"), ps[4 + d_][:],
                             maskf4 if d_ == 0 else maskb4, ALU.mult, [PK[4 + d_], "cf"], [f"attm{d_}"])
                      for h in range(4):
                          o_ = ps[6][:, h * 64:(h + 1) * 64]
                          vh = vall[:, c, h * 64:(h + 1) * 64]
                          mm(o_, attm[:, h, :], vh, ["attm0", f"v{c}"], [PK[6]], start=True, stop=False)
                          mm(o_, attm[:, 4 + h, :], vh, ["attm1", f"v{c}"], [PK[6]], start=False, stop=False)
                          mm(o_, m4[:, 2, h, :], stb16[:, c, 0:64], ["m42", "stb16"], [PK[6]], start=False, stop=False)
                          mm(o_, m4[:, 3, h, :], stb16[:, c, 64:128], ["m43", "stb16"], [PK[6]], start=False, stop=True)
                      act(osq[:], ps[6][:, 0:256], AF.Square, [PK[6]], ["osq"])
                      red(oss[:, 0:4], osq[:].rearrange("p (h n) -> p h n", h=4), ALU.add, ["osq"], ["oss"])
                      rstd_of(oss[:, 4:8], oss[:, 0:4], 64, ["oss"], ["orstd"])
                      tt("dve", o1[:].rearrange("p (h n) -> p h n", h=4), ps[6][:, 0:256].rearrange("p (h n) -> p h n", h=4),
                         oss[:, 4:8].unsqueeze(2).broadcast_to([128, 4, 64]), ALU.mult, [PK[6], "orstd"], ["o1"])
                      act(sg[:], ogall[:, c, :], AF.Silu, [f"og{c}"], ["sg"])
                      tt("pool", o1[:], o1[:], gng[:], ALU.mult, ["o1", "gng"], ["o1"])
                      tt("dve", ogl[:], o1[:], sg[:], ALU.mult, ["o1", "sg"], ["ogl"])
                      tpv = ps[7][:].bitcast(BF16)
                      for j in range(2):
                          tr(tpv[:, j * 128:(j + 1) * 128], ogl[:, j * 128:(j + 1) * 128], identb, ["ogl", "cb"], [PK[7]])
                      ob = c % 2
                      cp("act", oT[ob][:].rearrange("p k n -> p (k n)"), tpv[:, 0:256], [PK[7]], [f"oT{ob}"])
                      dma("sp", MIXT[0:256, t * 128:(t + 1) * 128].rearrange("(k p) n -> p k n", p=128), oT[ob][:], f"oTs{ob}",
                          r=[f"oT{ob}"])
                  S.barrier()
          S.barrier()

          if upto <= 2:
              raise _Stop()
          with ExitStack() as cx:
              NCm = Smax // 128
              xf = sb(cx, "xf", [128, NCm, 256], BF16)
              dbuf = [sb(cx, f"dbuf{i}", [128, NCm, 512], BF16) for i in range(2)]
              ab = sb(cx, "ab", [128, 2, 2, 512], BF16)
              fo = [sb(cx, f"fo{i}", [128, 2, 512], BF16) for i in range(2)]
              it = 0
              for si, Ss in enumerate(seqs):
                  NCs = Ss // 128
                  dma("sp", xf[:, 0:NCs, :], U[soff[si]:soff[si] + Ss, 1024:1280].rearrange("(c p) n -> p c n", p=128), "xf", w=["xf"])
                  for qb in range(Ss // 512):
                      for tg in range(2):
                          dma("sp", dbuf[tg][:, 0:NCs, :],
                              dft[Ss][tg].rearrange("(c p) n -> p c n", p=128)[:, :, qb * 512:(qb + 1) * 512], f"dft{tg}", w=[f"dbuf{tg}"])
                          for mc in range(2):
                              pb = tg * 2 + mc
                              for kc in range(NCs):
                                  mm(ps[pb][:], xf[:, kc, mc * 128:(mc + 1) * 128], dbuf[tg][:, kc, :], ["xf", f"dbuf{tg}"], [PK[pb]],
                                     start=(kc == 0), stop=(kc == NCs - 1))
                              cp("act" if mc == 0 else "dve", ab[:, tg, mc, :], ps[pb][:], [PK[pb]], [f"ab{tg}{mc}"])
                      ob = it % 2
                      it += 1
                      for mc in range(2):
                          mm(ps[4 + mc][:], cchan[:, 0:128], ab[:, 0, mc, :], ["cchan", f"ab0{mc}"], [PK[4 + mc]], start=True, stop=False)
                          mm(ps[4 + mc][:], cchan[:, 128:256], ab[:, 1, mc, :], ["cchan", f"ab1{mc}"], [PK[4 + mc]], start=False, stop=True)
                          cp("act" if mc == 0 else "dve", fo[ob][:, mc, :], ps[4 + mc][:], [PK[4 + mc]], [f"fo{ob}{mc}"])
                      c0 = soff[si] + qb * 512
                      dma("sp", MIXT[256:512, c0:c0 + 512].rearrange("(k p) n -> p k n", p=128), fo[ob][:], f"fos{ob}",
                          r=[f"fo{ob}0", f"fo{ob}1"])
          S.barrier()

          if upto <= 3:
              raise _Stop()
          with ExitStack() as cx:
              NCm = Smax // 128
              kT = sb(cx, "kT", [96, 8, Smax], BF16)
              vext = sb(cx, "vext", [128, NCm, 8, 80], BF16)
              cqT = sb(cx, "cqT", [128, 2, Smax], BF16)
              wuq = sb(cx, "wuq", [128, 2, 768], BF16)
              wukv = sb(cx, "wukv", [128, 1024], BF16)
              wq32 = sb(cx, "wq32", [128, 2, 768], F32)
              gq = sb(cx, "gq", [128, 8, 96], F32)
              gk = sb(cx, "gk", [128, 8, 96], F32)
              gql = sb(cx, "gql", [128, 256], F32)
              gkvl = sb(cx, "gkvl", [128, 128], F32)
              um = [sb(cx, f"um{i}", [128, 416], BF16) for i in range(2)]
              junk = sb(cx, "junk4", [128, 416], F32)
              junkB = sb(cx, "junkB", [128, 768], F32)
              st4 = sb(cx, "st4", [128, 32], F32)
              cqn = sb(cx, "cqn", [128, 384], BF16)
              ckT2 = [sb(cx, f"ckT{i}", [128, 128], BF16) for i in range(2)]
              kfull = sb(cx, "kfull", [128, 8, 96], BF16)
              ktmp = sb(cx, "ktmp", [128, 8, 96], F32)
              rtmp = sb(cx, "rtmp", [128, 4, 8, 16], F32)
              qT = [sb(cx, f"qT{i}", [96, 8, 512], BF16) for i in range(2)]
              pT = [sb(cx, f"pT{i}", [128, 512], BF16) for i in range(4)]
              osb = [sb(cx, f"osb{i}", [65, 512], F32) for i in range(2)]
              mlaT = [sb(cx, f"mlaT{i}", [64, 8, 512], BF16) for i in range(2)]
              dma("sp", wq32[:], w_uq[l].rearrange("(k p) n -> p k n", p=128), "p4c", w=["wq32"])
              cp("dve", wuq[:], wq32[:], ["wq32"], ["wuq"])
              dma("sp", wq32[:, 0, :], w_ukv[l][:, 0:768], "p4c", w=["wq32"], r=["wuq"])
              dma("sp", wq32[:, 1, 0:256], w_ukv[l][:, 768:1024], "p4c", w=["wq32"], r=["wuq"])
              cp("dve", wukv[:, 0:768], wq32[:, 0, :], ["wq32"], ["wukv"])
              cp("dve", wukv[:, 768:1024], wq32[:, 1, 0:256], ["wq32"], ["wukv"])
              for h in range(8):
                  dma("sp", gq[:, h, :], qn_g[l:l + 1, :].partition_broadcast(128), "p4c", w=["gq"])
                  dma("sp", gk[:, h, :], kn_g[l:l + 1, :].partition_broadcast(128), "p4c", w=["gk"])
              dma("sp", gql[:], qlora_g[l:l + 1, :].partition_broadcast(128), "p4c", w=["gql"])
              dma("sp", gkvl[:], kvlora_g[l:l + 1, :].partition_broadcast(128), "p4c", w=["gkvl"])
              memset("dve", vext[:].rearrange("p a b c -> p (a b c)"), 1.0, ["vones"])

              def rope(dst, src, tile_idx, rk, wk):
                  cosb = crope[:, tile_idx, 0:16].unsqueeze(1).broadcast_to([128, 8, 16])
                  sinb = crope[:, tile_idx, 16:32].unsqueeze(1).broadcast_to([128, 8, 16])
                  x1 = src[:, :, 64:80]
                  x2 = src[:, :, 80:96]
                  tt("dve", rtmp[:, 0], x1, cosb, ALU.mult, rk + ["crope"], ["rt0"])
                  tt("pool", rtmp[:, 1], x2, sinb, ALU.mult, rk + ["crope"], ["rt1"])
                  tt("dve", rtmp[:, 2], x1, sinb, ALU.mult, rk + ["crope"], ["rt2"])
                  tt("pool", rtmp[:, 3], x2, cosb, ALU.mult, rk + ["crope"], ["rt3"])
                  tt("dve", dst[:, :, 64:80], rtmp[:, 0], rtmp[:, 1], ALU.subtract, ["rt0", "rt1"], wk)
                  tt("dve", dst[:, :, 80:96], rtmp[:, 2], rtmp[:, 3], ALU.add, ["rt2", "rt3"], wk)

              it = 0
              sbank = 0
              for si, Ss in enumerate(seqs):
                  NCs = Ss // 128
                  t0 = soff[si] // 128
                  def kvgen(c, t, b_):
                      dma("sp", um[b_][:], U[t * 128:(t + 1) * 128, 1280:PU], f"um{b_}", w=[f"um{b_}"])
                      uk = f"um{b_}"
                      act(junk[:, 0:256], um[b_][:, 0:256], AF.Square, [uk], ["junk", "ssa"], accum_out=st4[:, 0:1])
                      act(junk[:, 256:384], um[b_][:, 256:384], AF.Square, [uk], ["junk", "ssb"], accum_out=st4[:, 1:2])
                      act(junk[:, 384:416], um[b_][:, 384:416], AF.Square, [uk], ["junk", f"ssc{b_}"], accum_out=st4[:, 24 + b_:25 + b_])
                      rstd_of(st4[:, 4:5], st4[:, 0:1], 256, ["ssa"], ["rsa"])
                      rstd_of(st4[:, 5:6], st4[:, 1:2], 128, ["ssb"], ["rsb"])
                      stt(cqn[:, 0:256], um[b_][:, 0:256], st4[:, 4:5], gql[:], ALU.mult, ALU.mult, [uk, "rsa", "gql"], ["cqn_a"])
                      stt(cqn[:, 256:384], um[b_][:, 256:384], st4[:, 5:6], gkvl[:], ALU.mult, ALU.mult, [uk, "rsb", "gkvl"], ["cqn_b"])
                      tpv = ps[7][:].bitcast(BF16)
                      for j in range(3):
                          tr(tpv[:, j * 128:(j + 1) * 128], cqn[:, j * 128:(j + 1) * 128], identb, ["cqn_a", "cqn_b", "cb"], [PK[7]])
                      cp("act", cqT[:, :, c * 128:(c + 1) * 128], tpv[:, 0:256].rearrange("p (k n) -> p k n", k=2), [PK[7]], [f"cqT{c}"])
                      cp("act", ckT2[b_][:], tpv[:, 256:384], [PK[7]], [f"ckT{b_}"])
                      yield
                      mm(ps[5][:], ckT2[b_][:], wukv[:, 0:512], [f"ckT{b_}", "wukv"], [PK[5]])
                      mm(ps[6][:], ckT2[b_][:], wukv[:, 512:1024], [f"ckT{b_}", "wukv"], [PK[6]])
                      for hf in range(2):
                          cp("act", vext[:, c, hf * 4:(hf + 1) * 4, 0:64],
                             ps[5 + hf][:].rearrange("p (h n) -> p h n", h=4)[:, :, 64:128], [PK[5 + hf], "vones"], [f"vx{c}_{hf}"])
                      for hf in range(2):
                          act(junkB[:, hf * 256:(hf + 1) * 256].rearrange("p (h n) -> p h n", h=4),
                              ps[5 + hf][:].rearrange("p (h n) -> p h n", h=4)[:, :, 0:64], AF.Square, [PK[5 + hf]], [f"junkk{hf}"])
                      red(st4[:, 8:16], junkB[:, 0:512].rearrange("p (h n) -> p h n", h=8), ALU.add, ["junkk0", "junkk1"], ["ssk"])
                      ts("dve", st4[:, 8:16], st4[:, 8:16], st4[:, 24 + b_:25 + b_], None, ALU.add, None, ["ssk", f"ssc{b_}"], ["ssk"])
                      rstd_of(st4[:, 16:24], st4[:, 8:16], 96, ["ssk"], ["rsk"])
                      for hf in range(2):
                          tt("dve", ktmp[:, hf * 4:(hf + 1) * 4, 0:64], ps[5 + hf][:].rearrange("p (h n) -> p h n", h=4)[:, :, 0:64],
                             st4[:, 16 + hf * 4:20 + hf * 4].unsqueeze(2).broadcast_to([128, 4, 64]), ALU.mult, [PK[5 + hf], "rsk"], [f"ktn{hf}"])
                      tt("dve", ktmp[:, :, 64:96], um[b_][:, 384:416].unsqueeze(1).broadcast_to([128, 8, 32]),
                         st4[:, 16:24].unsqueeze(2).broadcast_to([128, 8, 32]), ALU.mult, [uk, "rsk"], ["ktr"])
                      tt("pool", ktmp[:], ktmp[:], gk[:], ALU.mult, ["ktn0", "ktn1", "ktr", "gk"], ["ktg"])
                      cp("pool", kfull[:, :, 0:64], ktmp[:, :, 0:64], ["ktg"], ["kfa"])
                      rope(kfull, ktmp, c, ["ktg"], ["kfb"])
                      tpv = ps[4][:].bitcast(BF16)
                      for h in range(8):
                          tr(tpv[0:96, h * 128:(h + 1) * 128], kfull[:, h, :], identb, ["kfa", "kfb", "cb"], [PK[4]])
                      cp("act", kT[:, :, c * 128:(c + 1) * 128], tpv[0:96, :].rearrange("p (h n) -> p h n", h=8), [PK[4]], [f"kT{c}"])
                  prevk = None
                  for c in range(NCs):
                      t = t0 + c
                      b_ = it % 2
                      it += 1
                      g_ = kvgen(c, t, b_)
                      next(g_)
                      if prevk is not None:
                          for _ in prevk:
                              pass
                      prevk = g_
                  if prevk is not None:
                      for _ in prevk:
                          pass
                  S.barrier()
                  allk = []
                  allv = []
                  kflat = ktmp[:].rearrange("p h n -> p (h n)")

                  def qprep_gen(qb):
                      qbuf = qb % 2
                      for j in range(4):
                          c = qb * 4 + j
                          for kc in range(2):
                              mm(ps[5][:], cqT[:, kc, c * 128:(c + 1) * 128], wuq[:, kc, 0:512], ["wuq"], [PK[5]],
                                 start=(kc == 0), stop=(kc == 1))
                          for kc in range(2):
                              mm(ps[6][:, 0:256], cqT[:, kc, c * 128:(c + 1) * 128], wuq[:, kc, 512:768], ["wuq"], [PK[6]],
                                 start=(kc == 0), stop=(kc == 1))
                          yield
                          yield
                          cp("dve", kflat[:, 0:512], ps[5][:], [PK[5]], ["qa"])
                          cp("dve", kflat[:, 512:768], ps[6][:, 0:256], [PK[6]], ["qb"])
                          yield
                          yield
                          tt("dve", junkB[:, 0:768], kflat, kflat, ALU.mult, ["qa", "qb"], ["junkq"])
                          yield
                          yield
                          red(st4[:, 8:16], junkB[:, 0:768].rearrange("p (h n) -> p h n", h=8), ALU.add, ["junkq"], ["ssq"])
                          yield
                          yield
                          act(st4[:, 16:24], st4[:, 8:16], AF.Ln, ["ssq"], ["rsq"], scale=1.0 / 96, bias=EPS)
                          yield
                          act(st4[:, 16:24], st4[:, 16:24], AF.Exp, ["rsq"], ["rsq"], scale=-0.5)
                          yield
                          yield
                          tt("dve", ktmp[:], ktmp[:], st4[:, 16:24].unsqueeze(2).broadcast_to([128, 8, 96]), ALU.mult,
                             ["qa", "qb", "rsq"], ["qn"])
                          yield
                          yield
                          tt("pool", ktmp[:], ktmp[:], gq[:], ALU.mult, ["qn", "gq"], ["qg"])
                          yield
                          yield
                          yield
                          cp("pool", kfull[:, :, 0:64], ktmp[:, :, 0:64], ["qg"], ["kfa"])
                          rope(kfull, ktmp, c, ["qg"], ["kfb"])
                          yield
                          yield
                          yield
                          yield
                          tpv_ = ps[4][:].bitcast(BF16)
                          for h in range(8):
                              tr(tpv_[0:96, h * 128:(h + 1) * 128], kfull[:, h, :], identb, ["kfa", "kfb", "cb"], [PK[4]])
                          yield
                          yield
                          yield
                          cp("act", qT[qbuf][:, :, j * 128:(j + 1) * 128], tpv_[0:96, :].rearrange("p (h n) -> p h n", h=8),
                             [PK[4]], [f"qT{qbuf}_{j}"])
                          yield

                  def attn_gen(qb):
                      qbuf = qb % 2
                      qk = [f"qT{qbuf}_{j}" for j in range(4)]
                      steps = [(h, kc) for h in range(8) for kc in range(NCs)]
                      SBK = [0, 1]
                      Dp = 1

                      def qk_mm(i):
                          h_, kc_ = steps[i]
                          bk = SBK[i % len(SBK)]
                          mm(ps[bk][:], kT[:, h_, kc_ * 128:(kc_ + 1) * 128], qT[qbuf][:, h_, :], qk, [PK[bk]])

                      def ep2(h_):
                          a_ = h_ % 2
                          mm(ps[7][0:64, :], sel65, osb[a_][:], ["cf", f"osb{a_}"], [PK[7]])
                          tt("dve", mlaT[qbuf][:, h_, :], osb[a_][0:64, :], ps[7][0:64, :], ALU.mult, [f"osb{a_}", PK[7]],
                             [f"mlaT{qbuf}_{h_}"])

                      for i in range(min(Dp, len(steps))):
                          qk_mm(i)
                      due = {}
                      for i, (h, kc) in enumerate(steps):
                          ab_ = h % 2
                          acc = ps[2 + ab_]
                          bk = SBK[i % len(SBK)]
                          pb_ = i % 4
                          act(pT[pb_][:], ps[bk][:], AF.Exp, [PK[bk]], [f"pT{pb_}"], scale=96.0 ** -0.5)
                          if i + Dp < len(steps):
                              qk_mm(i + Dp)
                          mm(acc[0:65, :], vext[:, kc, h, 0:65], pT[pb_][:], [f"pT{pb_}"], [PK[2 + ab_]],
                             start=(kc == 0), stop=(kc == NCs - 1))
                          if kc == NCs - 1:
                              cp("dve", osb[ab_][:], acc[0:65, :], [PK[2 + ab_]], [f"osb{ab_}"])
                              recip(osb[ab_][64:65, :], osb[ab_][64:65, :], [f"osb{ab_}"], [f"osb{ab_}"])
                              due[i + 4] = h
                          if i in due:
                              ep2(due.pop(i))
                          yield
                      for i_ in sorted(due):
                          ep2(due[i_])
                      c0 = soff[si] + qb * 512
                      dma("sp", MIXT[512:1024, c0:c0 + 512].rearrange("(h p) n -> p h n", p=64), mlaT[qbuf][:], f"mls{qbuf}",
                          r=[f"mlaT{qbuf}_{h}" for h in range(8)])

                  nqb = Ss // 512 if os.environ.get('P4SKIP') != '1' else 0
                  if nqb:
                      for _ in qprep_gen(0):
                          pass
                  for qb in range(nqb):
                      pg = qprep_gen(qb + 1) if qb + 1 < nqb else None
                      for _ in attn_gen(qb):
                          if pg is not None:
                              try:
                                  next(pg)
                              except StopIteration:
                                  pg = None
                      if pg is not None:
                          for _ in pg:
                              pass
                  S.barrier()
          S.barrier()

          if upto <= 4:
              raise _Stop()
          with ExitStack() as cxm:
              m1all = sb(cxm, "m1all", [128, NT, 32], F32)
              m2all = sb(cxm, "m2all", [128, NT, 32], F32)
              rkall = sb(cxm, "rkall", [128, NT, 32], F32)
              w12 = sb(cxm, "w12", [128, NT, 2], F32)
              destf = sb(cxm, "destf", [128, NT, 2], F32)
              desti = sb(cxm, "desti", [128, NT, 2], I32)
              widx = sb(cxm, "widx", [128, NB, 12], I32)
              base = sb(cxm, "base", [128, 32], F32)
              eidx = cmoe[:, 0:32]
              thr = cmoe[:, 32:32 + NB]
              wstride = cmoe[:, 32 + NB:32 + NB + 12]
              wcst = cmoe[:, 32 + NB + 12:32 + NB + 24]
              with ExitStack() as cx:
                  wo32 = sb(cx, "wo32", [128, 4, D], F32)
                  wout = sb(cx, "wout", [128, 8, D], BF16)
                  wr = sb(cx, "wr", [128, 8, 36], F32)
                  rb = sb(cx, "rb", [128, 36], F32)
                  mixb = [sb(cx, f"mixb{i}", [128, 8, 512], BF16) for i in range(2)]
                  xt = [sb(cx, f"xt5{i}", [128, D], F32) for i in range(2)]
                  xn = [sb(cx, f"xn{i}", [128, D], F32) for i in range(2)]
                  h2f = [sb(cx, f"h2f{i}", [128, D], F32) for i in range(2)]
                  h2b = [sb(cx, f"h2b{i}", [128, D], BF16) for i in range(2)]
                  h2T = sb(cx, "h2T", [128, 8, 128], F32)
                  tmp5 = sb(cx, "tmp5", [128, D], F32)
                  junk5 = sb(cx, "junk5", [128, D], BF16)
                  modb = [sb(cx, f"modc{i}", [128, 3 * D], F32) for i in range(2)]
                  s5 = sb(cx, "s5", [128, 16], F32)
                  lg = sb(cx, "lg", [128, 36], F32)
                  lm = sb(cx, "lm", [128, 32], F32)
                  lm2 = sb(cx, "lm2", [128, 32], F32)
                  pen = sb(cx, "pen", [128, 4], F32)
                  gm = sb(cx, "gm", [128, 4], F32)
                  Mb = sb(cx, "Mb", [128, 32], BF16)
                  for half in range(2):
                      dma("sp", wo32[:], w_out[l].rearrange("(k p) n -> p k n", p=128)[:, half * 4:(half + 1) * 4, :], "p5w", w=["wo32"])
                      cp("dve", wout[:, half * 4:(half + 1) * 4, :], wo32[:], ["wo32"], [f"wout{half}"])
                  dma("sp", wr[:, :, 0:4], rg_w[l].rearrange("(k p) n -> p k n", p=128), "p5w", w=["wr"])
                  dma("sp", wr[:, :, 4:36], re_w[l].rearrange("(k p) n -> p k n", p=128), "p5w", w=["wr"])
                  dma("sp", rb[:, 0:4], rg_b[l:l + 1, :].partition_broadcast(128), "p5w", w=["rb"])
                  dma("sp", rb[:, 4:36], re_b[l:l + 1, :].partition_broadcast(128), "p5w", w=["rb"])
                  memset("dve", base[:], 0.0, ["base"])
                  tix = 0
                  prev5 = [None]
                  for si, Ss in enumerate(seqs):
                      mb = si % 2
                      dma("sp", modb[mb][:, 0:D], MOD[si:si + 1, 2 * D:3 * D].partition_broadcast(128), f"mc{mb}", w=[f"modc{mb}"])
                      dma("sp", modb[mb][:, D:3 * D], MOD[si:si + 1, 3 * D:5 * D].partition_broadcast(128), f"mc{mb}", w=[f"modc{mb}"])
                      for qb in range(Ss // 512):
                          xb = (soff[si] // 512 + qb) % 2
                          c0 = soff[si] + qb * 512
                          dma("sp", mixb[xb][:], MIXT[:, c0:c0 + 512].rearrange("(k p) n -> p k n", p=128), f"mixb{xb}", w=[f"mixb{xb}"])
                          def p5gen(t, b_, j, xb=xb, mb=mb):
                              dma("sp", xt[b_][:], x_src[t * 128:(t + 1) * 128, :], f"xt5{b_}", w=[f"xt5{b_}"])
                              for n in range(2):
                                  for k in range(8):
                                      mm(ps[n][:], mixb[xb][:, k, j * 128:(j + 1) * 128], wout[:, k, n * 512:(n + 1) * 512],
                                         [f"mixb{xb}", "wout0", "wout1"], [PK[n]], start=(k == 0), stop=(k == 7))
                              for n in range(2):
                                  tt("dve", tmp5[:, n * 512:(n + 1) * 512], ps[n][:], modb[mb][:, n * 512:(n + 1) * 512], ALU.mult,
                                     [PK[n], f"modc{mb}"], [f"tmp5{n}"])
                              tt("pool", xn[b_][:], tmp5[:], xt[b_][:], ALU.add, ["tmp50", "tmp51", f"xt5{b_}"], [f"xn{b_}"])
                              dma("sp", XM[t * 128:(t + 1) * 128, :], xn[b_][:], f"xns{b_}", r=[f"xn{b_}"])
                              act(junk5[:], xn[b_][:], AF.Square, [f"xn{b_}"], ["junk5", "ss5"], accum_out=s5[:, 0:1])
                              rstd_of(s5[:, 1:2], s5[:, 0:1], D, ["ss5"], ["rs5"])
                              stt(tmp5[:], xn[b_][:], s5[:, 1:2], modb[mb][:, 2 * D:3 * D], ALU.mult, ALU.mult,
                                  [f"xn{b_}", "rs5", f"modc{mb}"], ["tmp50", "tmp51"])
                              tt("dve", h2f[b_][:], tmp5[:], modb[mb][:, D:2 * D], ALU.add, ["tmp50", "tmp51", f"modc{mb}"], [f"h2f{b_}"])
                              cp("pool", h2b[b_][:], h2f[b_][:], [f"h2f{b_}"], [f"h2b{b_}"])
                              dma("sp", H2[t * 128:(t + 1) * 128, :], h2b[b_][:], f"h2s{b_}", r=[f"h2b{b_}"])
                              yield
                              for k in range(8):
                                  tr(ps[2 + k // 4][:, (k % 4) * 128:(k % 4 + 1) * 128], h2f[b_][:, k * 128:(k + 1) * 128], identf,
                                     [f"h2f{b_}", "cf"], [PK[2 + k // 4]])
                              cp("act", h2T[:, 0:4, :].rearrange("p k n -> p (k n)"), ps[2][:], [PK[2]], ["h2Ta"])
                              cp("dve", h2T[:, 4:8, :].rearrange("p k n -> p (k n)"), ps[3][:], [PK[3]], ["h2Tb"])
                              for k in range(8):
                                  mm(ps[4][:, 0:36], h2T[:, k, :], wr[:, k, :], ["h2Ta", "h2Tb", "wr"], [PK[4]], start=(k == 0), stop=(k == 7))
                              tt("dve", lg[:], ps[4][:, 0:36], rb[:], ALU.add, [PK[4], "rb"], ["lg"])
                              red(s5[:, 2:3], lg[:, 0:4], ALU.max, ["lg"], ["gmax"])
                              ts("dve", gm[:], lg[:, 0:4], s5[:, 2:3], None, ALU.is_equal, None, ["lg", "gmax"], ["gm"])
                              ts("dve", s5[:, 3:4], s5[:, 2:3], -1.0, None, ALU.mult, None, ["gmax"], ["ngmax"])
                              act(junk5[:, 0:4], lg[:, 0:4], AF.Exp, ["lg", "ngmax"], ["junk5", "gsum"], bias=s5[:, 3:4], accum_out=s5[:, 4:5])
                              recip(s5[:, 5:6], s5[:, 4:5], ["gsum"], ["gw"])
                              ts("dve", pen[:], gm[:], BIG, -BIG, ALU.mult, ALU.add, ["gm"], ["pen"])
                              tt("dve", lm[:].rearrange("p (g e) -> p g e", g=4), lg[:, 4:36].rearrange("p (g e) -> p g e", g=4),
                                 pen[:].unsqueeze(2).broadcast_to([128, 4, 8]), ALU.add, ["lg", "pen"], ["lm"])
                              red(s5[:, 6:7], lm[:], ALU.max, ["lm"], ["m1"])
                              ts("dve", m1all[:, t, :], lm[:], s5[:, 6:7], None, ALU.is_equal, None, ["lm", "m1"], [f"mk1_{t}"])
                              stt(lm2[:], m1all[:, t, :], -BIG, lm[:], ALU.mult, ALU.add, [f"mk1_{t}", "lm"], ["lm2"])
                              red(s5[:, 7:8], lm2[:], ALU.max, ["lm2"], ["m2"])
                              ts("dve", m2all[:, t, :], lm2[:], s5[:, 7:8], None, ALU.is_equal, None, ["lm2", "m2"], [f"mk2_{t}"])
                              tt("dve", s5[:, 8:9], s5[:, 7:8], s5[:, 6:7], ALU.subtract, ["m1", "m2"], ["dm"])
                              act(s5[:, 9:10], s5[:, 8:9], AF.Exp, ["dm"], ["edm"])
                              ts("dve", s5[:, 9:10], s5[:, 9:10], 1.0, None, ALU.add, None, ["edm"], ["edm"])
                              recip(s5[:, 10:11], s5[:, 9:10], ["edm"], ["p1"])
                              tt("dve", w12[:, t, 0:1], s5[:, 10:11], s5[:, 5:6], ALU.mult, ["p1", "gw"], [f"w1_{t}"])
                              tt("dve", w12[:, t, 1:2], s5[:, 5:6], w12[:, t, 0:1], ALU.subtract, ["gw", f"w1_{t}"], [f"w2_{t}"])
                              tt("dve", Mb[:], m1all[:, t, :], m2all[:, t, :], ALU.add, [f"mk1_{t}", f"mk2_{t}"], ["Mb"])
                              mm(ps[5][:, 0:32], Lstr, Mb[:], ["cb", "Mb"], [PK[5]])
                              mm(ps[5][:, 32:64], onesb, Mb[:], ["cb", "Mb"], [PK[5]])
                              tt("dve", rkall[:, t, :], ps[5][:, 0:32], base[:], ALU.add, [PK[5], "base"], [f"rk_{t}"])
                              tt("dve", base[:], ps[5][:, 32:64], base[:], ALU.add, [PK[5], "base"], ["base"])
                          for j in range(4):
                              t = c0 // 128 + j
                              b_ = tix % 2
                              tix += 1
                              g_ = p5gen(t, b_, j)
                              next(g_)
                              if prev5[0] is not None:
                                  for _ in prev5[0]:
                                      pass
                              prev5[0] = g_
                  if prev5[0] is not None:
                      for _ in prev5[0]:
                          pass
              S.barrier()
              with ExitStack() as cx:
                  cnt_ = sb(cx, "cnt_", [128, 32], F32)
                  pad_ = sb(cx, "pad_", [128, 32], F32)
                  pe_ = [sb(cx, f"pe_{i}", [128, 32], F32) for i in range(2)]
                  pst = sb(cx, "pst", [128, 32], F32)
                  cmpb = sb(cx, "cmpb", [128, NB, 32], F32)
                  bef = sb(cx, "bef", [128, NB], F32)
                  wif = sb(cx, "wif", [128, NB, 12], F32)
                  prk = sb(cx, "prk", [128, NT, 32], F32)
                  J = (2 * T) // BK
                  cmpj = sb(cx, "cmpj", [128, 32, J], F32)
                  tt("dve", cmpj[:], base[:].unsqueeze(2).broadcast_to([128, 32, J]), thr[:, 0:J].unsqueeze(1).broadcast_to([128, 32, J]),
                     ALU.is_gt, ["base", "cmoe"], ["cmpj"])
                  red(cnt_[:], cmpj[:], ALU.add, ["cmpj"], ["cnt_"])
                  ts("dve", pad_[:], cnt_[:], float(BK), None, ALU.mult, None, ["cnt_"], ["pad_"])
                  cp("dve", pe_[0][:], pad_[:], ["pad_"], ["pe_0"])
                  cur = 0
                  pk = ["pe_0"]
                  for sh in (1, 2, 4, 8, 16):
                      nx = 1 - cur
                      cp("dve", pe_[nx][:, 0:sh], pe_[cur][:, 0:sh], pk, [f"pe_{nx}a"])
                      tt("dve", pe_[nx][:, sh:32], pe_[cur][:, sh:32], pe_[cur][:, 0:32 - sh], ALU.add, pk, [f"pe_{nx}b"])
                      pk = [f"pe_{nx}a", f"pe_{nx}b"]
                      cur = nx
                  pend = pe_[cur]
                  tt("dve", pst[:], pend[:], pad_[:], ALU.subtract, pk + ["pad_"], ["pst"])
                  tt("dve", cmpb[:], pend[:].unsqueeze(1).broadcast_to([128, NB, 32]), thr.unsqueeze(2).broadcast_to([128, NB, 32]),
                     ALU.is_le, pk + ["cmoe"], ["cmpb"])
                  red(bef[:], cmpb[:], ALU.add, ["cmpb"], ["bef"])
                  ts("dve", bef[:], bef[:], 31.0, 32.0 * l, ALU.min, ALU.add, ["bef"], ["bef"])
                  tt("dve", wif[:], bef[:].unsqueeze(2).broadcast_to([128, NB, 12]), wstride.unsqueeze(1).broadcast_to([128, NB, 12]),
                     ALU.mult, ["bef", "cmoe"], ["wif"])
                  tt("dve", wif[:], wif[:], wcst.unsqueeze(1).broadcast_to([128, NB, 12]), ALU.add, ["wif", "cmoe"], ["wif"])
                  cp("dve", widx[:], wif[:], ["wif"], ["widx"])
                  allrk = [f"rk_{t}" for t in range(NT)]
                  allm1 = [f"mk1_{t}" for t in range(NT)]
                  allm2 = [f"mk2_{t}" for t in range(NT)]
                  tt("dve", prk[:], rkall[:], pst[:].unsqueeze(1).broadcast_to([128, NT, 32]), ALU.add, allrk + ["pst"], ["prk"])
                  tt("dve", rkall[:], prk[:], m1all[:], ALU.mult, ["prk"] + allm1 + allrk, ["rk1"])
                  red(destf[:, :, 0], rkall[:], ALU.add, ["rk1"], ["df0"])
                  tt("dve", rkall[:], prk[:], m2all[:], ALU.mult, ["prk", "rk1", "df0"] + allm2, ["rk2"])
                  red(destf[:, :, 1], rkall[:], ALU.add, ["rk2"], ["df1"])
                  cp("dve", desti[:], destf[:], ["df0", "df1"], ["desti"])
              S.barrier()
              stop5 = upto <= 5
              with ExitStack() as cx:
                  hb6 = [sb(cx, f"hb6{i}", [128, D], BF16) for i in range(4)]
                  for t in range(0 if stop5 else NT):
                      b_ = t % 4
                      dma("sp", hb6[b_][:], H2[t * 128:(t + 1) * 128, :], f"hb6{b_}", w=[f"hb6{b_}"])
                      scatter(HS[:, :], hb6[b_][:], desti[:, t, 0:1], f"sc{b_}", r=[f"hb6{b_}"])
                      scatter(HS[:, :], hb6[b_][:], desti[:, t, 1:2], f"sc{b_}", r=[f"hb6{b_}"])
              S.barrier()
              stop6 = upto <= 6
              with ExitStack() as cx:
                  w1b = [sb(cx, f"w1b{i}", [128, 8, 512], BF16) for i in range(2)]
                  w3b = [sb(cx, f"w3b{i}", [128, 8, 512], BF16) for i in range(2)]
                  w2b = [sb(cx, f"w2b{i}", [128, 4, D], BF16) for i in range(2)]
                  xs_ = [sb(cx, f"xs{i}", [128, 4, D], BF16) for i in range(2)]
                  xsT = sb(cx, "xsT", [128, 8, 512], BF16)
                  sgm = sb(cx, "sgm", [128, 512], F32)
                  hmT = sb(cx, "hmT", [128, 4, 512], BF16)
                  ybs = [sb(cx, f"ybs{i}", [128, 4, D], BF16) for i in range(2)]
                  w1rows = ew1.rearrange("l e (p a j) n -> (l e p a) (j n)", a=2, j=4)
                  w3rows = ew3.rearrange("l e (p a j) n -> (l e p a) (j n)", a=2, j=4)
                  w2rows = ew2.rearrange("l e (p a j) n -> (l e p a) (j n)", a=2, j=2)
                  for b in range(0 if (stop5 or stop6) else NB):
                      b_ = b % 2
                      for a_ in range(2):
                          gather(w1b[b_][:].rearrange("p k n -> p (k n)")[:, a_ * 2048:(a_ + 1) * 2048], w1rows, widx[:, b, a_:a_ + 1],
                                 f"gw{b_}", r=["widx"], w=[f"w1b{b_}_{a_}"])
                          gather(w3b[b_][:].rearrange("p k n -> p (k n)")[:, a_ * 2048:(a_ + 1) * 2048], w3rows, widx[:, b, a_:a_ + 1],
                                 f"gw{b_}", r=["widx"], w=[f"w3b{b_}_{a_}"])
                          gather(w2b[b_][:].rearrange("p k n -> p (k n)")[:, a_ * 2048:(a_ + 1) * 2048], w2rows, widx[:, b, a_:a_ + 1],
                                 f"gw{b_}", r=["widx"], w=[f"w2b{b_}_{a_}"])
                      dma("sp", xs_[b_][:], HS[b * BK:(b + 1) * BK, :].rearrange("(j p) n -> p j n", p=128), f"xs{b_}", w=[f"xs{b_}"])
                      for hk in range(2):
                          for kk in range(4):
                              k = hk * 4 + kk
                              tpv = ps[kk // 2][:].bitcast(BF16)
                              for j in range(4):
                                  tr(tpv[:, (kk % 2) * 512 + j * 128:(kk % 2) * 512 + (j + 1) * 128], xs_[b_][:, j, k:D:8],
                                     identb, [f"xs{b_}", "cb"], [PK[kk // 2]])
                          for q_ in range(2):
                              cp("act", xsT[:, hk * 4 + q_ * 2:hk * 4 + q_ * 2 + 2, :].rearrange("p k n -> p (k n)"),
                                 ps[q_][:].bitcast(BF16), [PK[q_]], [f"xsT{hk}{q_}"])
                      xk = [f"xsT{a}{c_}" for a in range(2) for c_ in range(2)]
                      for m in range(4):
                          pa = ps[2 + (m % 2) * 2]
                          pb2 = ps[3 + (m % 2) * 2]
                          ka, kb = PK[2 + (m % 2) * 2], PK[3 + (m % 2) * 2]
                          for k in range(8):
                              mm(pa[:], w1b[b_][:, k, m:512:4], xsT[:, k, :], [f"w1b{b_}_0", f"w1b{b_}_1"] + xk, [ka], start=(k == 0), stop=(k == 7))
                          for k in range(8):
                              mm(pb2[:], w3b[b_][:, k, m:512:4], xsT[:, k, :], [f"w3b{b_}_0", f"w3b{b_}_1"] + xk, [kb], start=(k == 0), stop=(k == 7))
                          act(sgm[:], pa[:], AF.Silu, [ka], ["sgm"])
                          tt("dve", hmT[:, m, :], sgm[:], pb2[:], ALU.mult, ["sgm", kb], [f"hmT{m}"])
                      hk_ = [f"hmT{m}" for m in range(4)]
                      for j in range(4):
                          for n in range(2):
                              pq = 6 + n
                              for m in range(4):
                                  mm(ps[pq][:], hmT[:, m, j * 128:(j + 1) * 128], w2b[b_][:, m, n * 512:(n + 1) * 512],
                                     hk_ + [f"w2b{b_}_0", f"w2b{b_}_1"], [PK[pq]], start=(m == 0), stop=(m == 3))
                              cp("act" if n == 0 else "dve", ybs[b_][:, j, n * 512:(n + 1) * 512], ps[pq][:], [PK[pq]], [f"ybs{b_}_{j}{n}"])
                      dma("sp", YB[b * BK:(b + 1) * BK, :].rearrange("(j p) n -> p j n", p=128), ybs[b_][:], f"ybst{b_}",
                          r=[f"ybs{b_}_{j}{n}" for j in range(4) for n in range(2)])
              S.barrier()
              stop7 = upto <= 7
              with ExitStack() as cx:
                  y1 = [sb(cx, f"y1_{i}", [128, D], BF16) for i in range(2)]
                  y2 = [sb(cx, f"y2_{i}", [128, D], BF16) for i in range(2)]
                  xt = [sb(cx, f"xt8{i}", [128, D], F32) for i in range(2)]
                  xo = [sb(cx, f"xo{i}", [128, D], F32) for i in range(2)]
                  t8 = [sb(cx, f"t8_{i}", [128, D], F32) for i in range(2)]
                  t9 = [sb(cx, f"t9_{i}", [128, D], F32) for i in range(2)]
                  g2b = [sb(cx, f"g2b{i}", [128, D], F32) for i in range(2)]
                  for si, Ss in enumerate([] if (stop5 or stop6 or stop7) else seqs):
                      mb = si % 2
                      dma("sp", g2b[mb][:], MOD[si:si + 1, 5 * D:6 * D].partition_broadcast(128), f"g2{mb}", w=[f"g2b{mb}"])
                      for tl in range(Ss // 128):
                          t = soff[si] // 128 + tl
                          b_ = t % 2
                          gather(y1[b_][:], YB[:, :], desti[:, t, 0:1], f"gy{b_}", r=["desti"], w=[f"y1_{b_}"])
                          gather(y2[b_][:], YB[:, :], desti[:, t, 1:2], f"gy{b_}", r=["desti"], w=[f"y2_{b_}"])
                          dma("sp", xt[b_][:], XM[t * 128:(t + 1) * 128, :], f"xt8{b_}", w=[f"xt8{b_}"])
                          act(t8[b_][:], y1[b_][:], AF.Copy, [f"y1_{b_}"], [f"t8{b_}"], scale=w12[:, t, 0:1])
                          stt(t9[b_][:], y2[b_][:], w12[:, t, 1:2], t8[b_][:], ALU.mult, ALU.add, [f"y2_{b_}", f"t8{b_}"], [f"t9{b_}"])
                          tt("dve", t9[b_][:], t9[b_][:], g2b[mb][:], ALU.mult, [f"t9{b_}", f"g2b{mb}"], [f"t9{b_}"])
                          tt("dve", xo[b_][:], t9[b_][:], xt[b_][:], ALU.add, [f"t9{b_}", f"xt8{b_}"], [f"xo{b_}"])
                          dma("sp", x_dst[t * 128:(t + 1) * 128, :], xo[b_][:], f"xos{b_}", r=[f"xo{b_}"])
              S.barrier()
          x_src = XR

    except _Stop:
        pass
    nops = S.emit()
    return nc, stack, nops, NB


def host_consts(seqs):
    T = sum(seqs)
    NB = (2 * T) // BK + NEXP
    Smax = max(seqs)
    bf = ml_dtypes.bfloat16
    p = np.arange(128)
    cf = np.zeros((128, 1792), np.float32)
    cf[:, 0:128] = np.eye(128)
    cf[:, 128:256] = (p[:, None] <= p[None, :])
    cf[:, 256:384] = (p[:, None] >= p[None, :])
    cf[:, 384:512] = 1.0
    mf = (p[:, None] <= p[None, :]).astype(np.float32)
    mb = (p[:, None] > p[None, :]).astype(np.float32)
    cf[:, 512:1024] = np.tile(mf, (1, 4))
    cf[:, 1024:1536] = np.tile(mb, (1, 4))
    cf[64, 1536:1600] = 1.0
    cb = np.zeros((128, 1408), np.float32)
    cb[:, 0:128] = np.eye(128)
    cb[:, 128:256] = 1.0
    cb[:, 256:384] = (p[:, None] < p[None, :])
    hm = np.zeros((128, 4, 128), np.float32)
    for h in range(4):
        hm[32 * h:32 * (h + 1), h, :] = 1.0
    cb[:, 384:896] = hm.reshape(128, 512)
    hc = np.zeros((128, 4, 128), np.float32)
    for h in range(4):
        hc[:, h, 32 * h:32 * (h + 1)] = 1.0
    cb[:, 896:1408] = hc.reshape(128, 512)
    half = 16
    freqs = 10000.0 ** (-np.arange(half, dtype=np.float32) / half)
    pos = np.arange(Smax, dtype=np.float32)
    ang = pos[:, None] * freqs[None, :]
    rope = np.concatenate([np.cos(ang), np.sin(ang)], axis=1).astype(np.float32)
    rope = rope.reshape(Smax // 128, 128, 32).transpose(1, 0, 2).copy()
    cm = np.zeros((128, 32 + NB + 24), np.float32)
    cm[:, 0:32] = np.arange(32)[None, :]
    cm[:, 32:32 + NB] = (np.arange(NB) * BK)[None, :]
    cm[:, 32 + NB:32 + NB + 8] = 256.0
    cm[:, 32 + NB + 8:32 + NB + 12] = 512.0
    cm[:, 32 + NB + 12:32 + NB + 24] = 2 * p[:, None]
    cm[:, 32 + NB + 13] += 1
    c64 = np.arange(64)
    a64 = 2 * np.pi * np.outer(c64, c64) / 64.0
    Cc = np.cos(a64) / 8.0
    Sc = np.sin(a64) / 8.0
    cch = np.zeros((128, 256), np.float32)
    for g in range(2):
        cch[64 * g:64 * (g + 1), 64 * g:64 * (g + 1)] = Cc
        cch[64 * g:64 * (g + 1), 128 + 64 * g:128 + 64 * (g + 1)] = -Sc
    out = {"c_f32": cf, "c_bf": cb.astype(bf), "c_rope": rope, "c_moe": cm, "c_chan": cch.astype(bf)}
    for s_ in sorted(set(seqs)):
        n = np.arange(s_, dtype=np.int64)
        m = np.outer(n, n) % s_
        a = 2 * np.pi * m.astype(np.float64) / s_
        sc = 1.0 / np.sqrt(s_)
        out[f"dftc{s_}"] = (np.cos(a) * sc).astype(np.float32).astype(bf)
        out[f"dfts{s_}"] = (np.sin(a) * sc).astype(np.float32).astype(bf)
    return out


WNAMES = ["ada_w", "ada_b", "norm1_g", "norm2_g", "w_in", "w_out", "gla_gate_up_f", "gla_gate_bias_f", "gla_gate_up_b",
          "gla_gate_bias_b", "gla_out_norm_g", "mla_q_lora_norm_g", "mla_w_uq", "mla_kv_lora_norm_g", "mla_w_ukv",
          "mla_q_norm_g", "mla_k_norm_g", "router_group_w", "router_group_b", "router_expert_w", "router_expert_b",
          "expert_w1", "expert_w3", "expert_w2"]


def run(xs_per_core, cs_per_core, weights, seqs, depth, debug=False):
    nc, stack, nops, NB = build(seqs, depth, debug=debug)
    consts = host_consts(seqs)
    in_maps = []
    for xc, cc in zip(xs_per_core, cs_per_core):
        m = {"x_all": xc, "cT": cc}
        for k in WNAMES:
            m[k] = weights[k]
        m.update(consts)
        in_maps.append(m)
    res = run_bass_kernel_spmd(nc, in_maps, core_ids=list(range(len(in_maps))))
    stack.close()
    return res


def kernel(x_prompt, x_sample, c_prompt, c_sample, **weights):
    ncore = 8
    xp = np.asarray(x_prompt, np.float32)
    xs = np.asarray(x_sample, np.float32)
    cp_ = np.asarray(c_prompt, np.float32)
    cs_ = np.asarray(c_sample, np.float32)
    Bp, Sp, _ = xp.shape
    Bs, Ss, _ = xs.shape
    npc, nsc = Bp // ncore, Bs // ncore
    seqs = [Sp] * npc + [Ss] * nsc
    depth = np.asarray(weights["ada_w"]).shape[0]
    w = {k: np.ascontiguousarray(np.asarray(weights[k], np.float32)) for k in WNAMES}
    xcs, ccs = [], []
    for i in range(ncore):
        xcs.append(np.ascontiguousarray(np.concatenate(
            [xp[i * npc:(i + 1) * npc].reshape(-1, D), xs[i * nsc:(i + 1) * nsc].reshape(-1, D)], axis=0)))
        ccs.append(np.ascontiguousarray(np.concatenate([cp_[i * npc:(i + 1) * npc], cs_[i * nsc:(i + 1) * nsc]], axis=0).T))
    res = run(xcs, ccs, w, seqs, depth)
    yp = np.zeros_like(xp)
    ys = np.zeros_like(xs)
    for i in range(ncore):
        y = res.results[i]["y_all"]
        yp[i * npc:(i + 1) * npc] = y[:npc * Sp].reshape(npc, Sp, D)
        ys[i * nsc:(i + 1) * nsc] = y[npc * Sp:].reshape(nsc, Ss, D)
    return (yp, ys)
```
